# Optimizing a Trainium2 kernel written in Bass

```python
import math
import jax, jax.numpy as jnp
from jax import lax
import numpy as np

D_MODEL = 1024
BATCH = 4
SEQ = 4096
DEPTH = 2

HEAD_DIM = 64
N_SB_HEADS = 8
N_MOBA_HEADS = 8
N_ATT_HEADS = N_SB_HEADS + N_MOBA_HEADS
ATT_W = N_ATT_HEADS * HEAD_DIM
SB_QBLOCK = 128
MOBA_BLOCK = 256
MOBA_TOPK = 3
MOBA_QBLOCK = 32
N_RET_HEADS = 4
RET_DK = D_MODEL // N_RET_HEADS
RET_DV = 2 * RET_DK
RET_QK_W = N_RET_HEADS * RET_DK
RET_V_W = N_RET_HEADS * RET_DV
RET_CHUNK = 128
D_FF = -(-8 * D_MODEL // 768) * 256
ROPE_THETA = 10000.0
NORM_EPS = 1e-6
GN_EPS = 1e-5
NEG = -1e30
N_EVEN = (DEPTH + 1) // 2
N_ODD = DEPTH // 2

kernel_name = "hybrid_stickbreak_moba_retention_adaln"


def rms_norm(x, g):
    xf = x.astype(jnp.float32)
    y = xf * lax.rsqrt(jnp.mean(xf * xf, axis=-1, keepdims=True) + NORM_EPS)
    return (y * g.astype(jnp.float32)).astype(x.dtype)


def rotary(x, pos):
    d = x.shape[-1]
    inv = ROPE_THETA ** (-jnp.arange(0, d, 2, dtype=jnp.float32) / d)
    ang = pos.astype(jnp.float32)[:, None] * inv[None, :]
    cos, sin = jnp.cos(ang), jnp.sin(ang)
    xf = x.astype(jnp.float32)
    x1, x2 = xf[..., : d // 2], xf[..., d // 2:]
    out = jnp.concatenate([x1 * cos - x2 * sin, x2 * cos + x1 * sin], axis=-1)
    return out.astype(x.dtype)


def ada_mod(c, w, b):
    m = jax.nn.silu(c) @ w + b
    return jnp.split(m[:, None, :], 6, axis=-1)


def modulate(h, shift, scale):
    return h * (1 + scale) + shift


def stick_breaking_attention(q, k, v):
    B, H, S, D = q.shape
    nblk = S // SB_QBLOCK
    kpos = jnp.arange(S)
    scale = D ** -0.5

    def block(i):
        start = i * SB_QBLOCK
        qb = lax.dynamic_slice_in_dim(q, start, SB_QBLOCK, axis=2)
        z = jnp.einsum('bhqd,bhkd->bhqk', qb, k).astype(jnp.float32) * scale
        qpos = start + jnp.arange(SB_QBLOCK)
        strict = kpos[None, :] < qpos[:, None]
        log_1mb = jnp.where(strict, jax.nn.log_sigmoid(-z), 0.0)
        after = lax.cumsum(log_1mb, axis=3, reverse=True) - log_1mb
        a = jnp.where(strict, jnp.exp(jax.nn.log_sigmoid(z) + after), 0.0)
        return jnp.einsum('bhqk,bhkd->bhqd', a.astype(v.dtype), v)

    out = lax.map(block, jnp.arange(nblk))
    return jnp.moveaxis(out, 0, 2).reshape(B, H, S, D)


def moba_attention(q, k, v):
    B, H, S, D = q.shape
    nb = -(-S // MOBA_BLOCK)
    spad = nb * MOBA_BLOCK
    pad = ((0, 0), (0, 0), (0, spad - S), (0, 0))
    kp = jnp.pad(k, pad)
    vp = jnp.pad(v, pad)
    kb = kp.reshape(B, H, nb, MOBA_BLOCK, D)
    vb = vp.reshape(B, H, nb, MOBA_BLOCK, D)
    kmean = jnp.mean(kb.astype(jnp.float32), axis=3).astype(k.dtype)
    kk = min(MOBA_TOPK, nb)
    scale = D ** -0.5
    nqb = S // MOBA_QBLOCK
    bi = jnp.arange(B)[:, None, None, None]
    hi = jnp.arange(H)[None, :, None, None]
    blk_ids = jnp.arange(nb)
    kpos_in = jnp.arange(MOBA_BLOCK)

    def block(i):
        start = i * MOBA_QBLOCK
        qb = lax.dynamic_slice_in_dim(q, start, MOBA_QBLOCK, axis=2)
        qpos = start + jnp.arange(MOBA_QBLOCK)
        own = start // MOBA_BLOCK
        gate = jnp.einsum('bhqd,bhnd->bhqn', qb, kmean).astype(jnp.float32)
        gate = jnp.where(blk_ids < own, gate, NEG)
        _, idx = lax.top_k(gate, kk)
        sel_valid = idx < own
        ksel = kb[bi, hi, idx]
        vsel = vb[bi, hi, idx]
        s_past = jnp.einsum('bhqd,bhqnkd->bhqnk', qb, ksel).astype(jnp.float32) * scale
        s_past = jnp.where(sel_valid[..., None], s_past, NEG)
        s_past = s_past.reshape(B, H, MOBA_QBLOCK, kk * MOBA_BLOCK)
        k_own = lax.dynamic_slice_in_dim(kp, own * MOBA_BLOCK, MOBA_BLOCK, axis=2)
        v_own = lax.dynamic_slice_in_dim(vp, own * MOBA_BLOCK, MOBA_BLOCK, axis=2)
        s_own = jnp.einsum('bhqd,bhkd->bhqk', qb, k_own).astype(jnp.float32) * scale
        kpos_own = own * MOBA_BLOCK + kpos_in
        s_own = jnp.where(kpos_own[None, :] <= qpos[:, None], s_own, NEG)
        p = jax.nn.softmax(jnp.concatenate([s_past, s_own], axis=-1), axis=-1).astype(v.dtype)
        p_past = p[..., : kk * MOBA_BLOCK].reshape(B, H, MOBA_QBLOCK, kk, MOBA_BLOCK)
        p_own = p[..., kk * MOBA_BLOCK:]
        return (jnp.einsum('bhqnk,bhqnkd->bhqd', p_past, vsel)
                + jnp.einsum('bhqk,bhkd->bhqd', p_own, v_own))

    out = lax.map(block, jnp.arange(nqb))
    return jnp.moveaxis(out, 0, 2).reshape(B, H, S, D)


def retention_chunkwise(q, k, v):
    B, H, S, dk = q.shape
    dv = v.shape[-1]
    C = RET_CHUNK
    nc = S // C
    log_g = jnp.log(1.0 - 2.0 ** (-5.0 - jnp.arange(H, dtype=jnp.float32)))
    n = jnp.arange(C, dtype=jnp.float32)
    diff = n[:, None] - n[None, :]
    decay = jnp.where(diff >= 0, jnp.exp(log_g[:, None, None] * jnp.maximum(diff, 0.0)), 0.0)
    xi = jnp.exp(log_g[:, None] * (n + 1.0))
    zeta = jnp.exp(log_g[:, None] * (C - 1.0 - n))
    chunk_decay = jnp.exp(log_g * C)

    def chunks(t):
        return jnp.moveaxis(t.reshape(B, H, nc, C, t.shape[-1]), 2, 0)

    def step(state, inp):
        qi, ki, vi = inp
        inner = jnp.einsum('bhqd,bhkd->bhqk', qi, ki).astype(jnp.float32) * decay
        o = (jnp.einsum('bhqk,bhkv->bhqv', inner, vi.astype(jnp.float32))
             + jnp.einsum('bhqd,bhdv->bhqv', qi.astype(jnp.float32), state) * xi[..., None])
        state = state * chunk_decay[:, None, None] + jnp.einsum(
            'bhkd,bhkv->bhdv', ki.astype(jnp.float32) * zeta[..., None], vi.astype(jnp.float32))
        return state, o

    init = jnp.zeros((B, H, dk, dv), jnp.float32)
    _, out = lax.scan(step, init, (chunks(q), chunks(k), chunks(v)))
    return jnp.moveaxis(out, 0, 2).reshape(B, H, S, dv)


def attn_hybrid_mixer(h, w_qkv, w_o, pos):
    B, S, _ = h.shape
    qkv = (h @ w_qkv).reshape(B, S, 3, N_ATT_HEADS, HEAD_DIM).transpose(2, 0, 3, 1, 4)
    q, k, v = qkv[0], qkv[1], qkv[2]
    sb = stick_breaking_attention(q[:, :N_SB_HEADS], k[:, :N_SB_HEADS], v[:, :N_SB_HEADS])
    qm = rotary(q[:, N_SB_HEADS:], pos)
    km = rotary(k[:, N_SB_HEADS:], pos)
    mb = moba_attention(qm, km, v[:, N_SB_HEADS:])
    o = jnp.concatenate([sb, mb], axis=1).transpose(0, 2, 1, 3).reshape(B, S, ATT_W)
    return o @ w_o


def retention_mixer(h, w_in, gn_gain, w_o, pos):
    B, S, _ = h.shape
    proj = h @ w_in
    q, k, v, g = jnp.split(proj, [RET_QK_W, 2 * RET_QK_W, 2 * RET_QK_W + RET_V_W], axis=-1)

    def heads(t, d):
        return t.reshape(B, S, N_RET_HEADS, d).transpose(0, 2, 1, 3)

    q = rotary(heads(q, RET_DK), pos)
    k = rotary(heads(k, RET_DK), pos) * (RET_DK ** -0.5)
    v = heads(v, RET_DV)
    o = retention_chunkwise(q, k, v)
    mu = jnp.mean(o, axis=-1, keepdims=True)
    var = jnp.mean(jnp.square(o - mu), axis=-1, keepdims=True)
    o = (o - mu) * lax.rsqrt(var + GN_EPS)
    o = o.transpose(0, 2, 1, 3).reshape(B, S, RET_V_W) * gn_gain.astype(jnp.float32)
    return (jax.nn.silu(g) * o.astype(h.dtype)) @ w_o


def swiglu(h, w_gate_up, w_down):
    gt, up = jnp.split(h @ w_gate_up, 2, axis=-1)
    return (jax.nn.silu(gt) * up) @ w_down


def setup_inputs(seed: int = 0) -> dict:
    key = jax.random.key(seed)
    ks = jax.random.split(key, 14)
    f32 = jnp.float32

    def nrm(k, shape, fan_in):
        return jax.random.normal(k, shape, f32) * (fan_in ** -0.5)

    return {
        "x": jax.random.normal(ks[0], (BATCH, SEQ, D_MODEL), f32),
        "c": jax.random.normal(ks[1], (BATCH, D_MODEL), f32),
        "ada_w": nrm(ks[2], (DEPTH, D_MODEL, 6 * D_MODEL), D_MODEL),
        "ada_b": 0.02 * jax.random.normal(ks[3], (DEPTH, 6 * D_MODEL), f32),
        "norm_gains": 1.0 + 0.05 * jax.random.normal(ks[4], (DEPTH, 2, D_MODEL), f32),
        "att_w_qkv": nrm(ks[5], (N_EVEN, D_MODEL, 3 * ATT_W), D_MODEL),
        "att_w_o": nrm(ks[6], (N_EVEN, ATT_W, D_MODEL), ATT_W),
        "ret_w_in": nrm(ks[7], (N_ODD, D_MODEL, 2 * RET_QK_W + 2 * RET_V_W), D_MODEL),
        "ret_gn": 1.0 + 0.05 * jax.random.normal(ks[8], (N_ODD, RET_V_W), f32),
        "ret_w_o": nrm(ks[9], (N_ODD, RET_V_W, D_MODEL), RET_V_W),
        "ffn_w_gate_up": nrm(ks[10], (DEPTH, D_MODEL, 2 * D_FF), D_MODEL),
        "ffn_w_down": nrm(ks[11], (DEPTH, D_FF, D_MODEL), D_FF),
        "final_norm": 1.0 + 0.05 * jax.random.normal(ks[12], (D_MODEL,), f32),
    }


def reference(x, c, ada_w, ada_b, norm_gains, att_w_qkv, att_w_o, ret_w_in, ret_gn,
              ret_w_o, ffn_w_gate_up, ffn_w_down, final_norm):
    S = x.shape[1]
    pos = jnp.arange(S, dtype=jnp.int32)
    for layer in range(DEPTH):
        shift_m, scale_m, gate_m, shift_f, scale_f, gate_f = ada_mod(c, ada_w[layer], ada_b[layer])
        h = modulate(rms_norm(x, norm_gains[layer, 0]), shift_m, scale_m)
        j = layer // 2
        if layer % 2 == 0:
            y = attn_hybrid_mixer(h, att_w_qkv[j], att_w_o[j], pos)
        else:
            y = retention_mixer(h, ret_w_in[j], ret_gn[j], ret_w_o[j], pos)
        x = x + gate_m * y
        h = modulate(rms_norm(x, norm_gains[layer, 1]), shift_f, scale_f)
        x = x + gate_f * swiglu(h, ffn_w_gate_up[layer], ffn_w_down[layer])
    return rms_norm(x, final_norm)
```

```python
import contextlib
import math
import numpy as np
import ml_dtypes
import concourse.bass as bass
import concourse.mybir as mybir
from concourse.bass_utils import run_bass_kernel_spmd

F32 = mybir.dt.float32
BF16 = mybir.dt.bfloat16
AF = mybir.ActivationFunctionType
ALU = mybir.AluOpType
AX = mybir.AxisListType

S_LEN = 4096
D = 1024
NT = S_LEN // 128
NQC = S_LEN // 512
DFF = 2816
NFC = DFF // 128
BIG = 30000.0
PI = math.pi


class Buf:
    __slots__ = ("name", "w", "r")

    def __init__(self, name=""):
        self.name = name
        self.w = None
        self.r = {}


class Op:
    __slots__ = ("eng", "is_dma", "sem", "val", "idx")


class Sched:
    def __init__(self, nc, es, n_dma_sems=32, epoch=30000):
        self.nc = nc
        self.es = es
        self.engs = {"pe": nc.tensor, "act": nc.scalar, "dve": nc.vector, "pool": nc.gpsimd, "sp": nc.sync}
        self.n = 0
        self.epoch = epoch
        self.cnt = {e: 0 for e in ("pe", "act", "dve", "pool")}
        self.cur_sem = {}
        for e in self.cnt:
            self.cur_sem[e] = es.enter_context(nc.semaphore("s_" + e + "0"))
        self.nsem = {e: 1 for e in self.cnt}
        self.dma_sems = [es.enter_context(nc.semaphore("s_dma%d" % i)) for i in range(n_dma_sems)]
        self.dma_last = [None] * n_dma_sems
        self.dma_cnt = [0] * n_dma_sems
        self.dma_rr = 0
        self.n_hw = n_dma_sems
        self.waited = {}
        self.pending_pe = []
        self.last_op = {}
        self.out_dmas = []
        self.n_wait = 0

    def _emit_wait(self, eng, d):
        assert d.sem is not None, "dependency on un-signalled PE op"
        key = (eng, id(d.sem))
        if self.waited.get(key, -1) >= d.val:
            return
        self.waited[key] = d.val
        self.engs[eng].wait_ge(d.sem, d.val)
        self.n_wait += 1

    def _deps(self, eng, reads, writes, is_dma):
        deps = {}
        for b in reads:
            if b.w is not None:
                deps[b.w.idx] = b.w
        for b in writes:
            if b.w is not None:
                deps[b.w.idx] = b.w
            for r in b.r.values():
                deps[r.idx] = r
        for d in deps.values():
            if (not d.is_dma) and d.eng == "pe" and eng == "pe" and not is_dma:
                continue
            self._emit_wait(eng, d)

    def _record(self, op, eng, reads, writes):
        rkey = ("dma", op.idx) if op.is_dma else eng
        for b in reads:
            b.r[rkey] = op
        for b in writes:
            b.w = op
            b.r = {}
        self.last_op[rkey if not op.is_dma else ("dmaq", eng)] = op

    def op(self, eng, fn, reads=(), writes=(), sig=True):
        self._deps(eng, reads, writes, False)
        inst = fn()
        op = Op()
        op.eng = eng
        op.is_dma = False
        op.idx = self.n
        self.n += 1
        op.sem = None
        op.val = None
        if eng == "pe" and not sig:
            self.pending_pe.append(op)
        else:
            if self.cnt[eng] >= self.epoch:
                self.cur_sem[eng] = self.es.enter_context(self.nc.semaphore("s_%s%d" % (eng, self.nsem[eng])))
                self.nsem[eng] += 1
                self.cnt[eng] = 0
            self.cnt[eng] += 1
            op.sem = self.cur_sem[eng]
            op.val = self.cnt[eng]
            inst.then_inc(op.sem, 1)
            if eng == "pe":
                for p in self.pending_pe:
                    p.sem = op.sem
                    p.val = op.val
                self.pending_pe = []
        self._record(op, eng, reads, writes)
        return op

    def pe(self, fn, r=(), w=(), sig=False):
        return self.op("pe", fn, r, w, sig)

    def act(self, fn, r=(), w=()):
        return self.op("act", fn, r, w)

    def dve(self, fn, r=(), w=()):
        return self.op("dve", fn, r, w)

    def pool(self, fn, r=(), w=()):
        return self.op("pool", fn, r, w)

    def dma(self, q, fn, reads=(), writes=(), is_out=False):
        if q == "pool":
            self.dma_sems.append(self.es.enter_context(self.nc.semaphore("s_sw%d" % len(self.dma_sems))))
            self.dma_last.append(None)
            self.dma_cnt.append(0)
            slot = len(self.dma_sems) - 1
        else:
            slot = self.dma_rr
            self.dma_rr = (self.dma_rr + 1) % self.n_hw
        prev = self.dma_last[slot]
        if prev is not None:
            self._emit_wait(q, prev)
        self._deps(q, reads, writes, True)
        inst = fn()
        op = Op()
        op.eng = q
        op.is_dma = True
        op.idx = self.n
        self.n += 1
        self.dma_cnt[slot] += 16
        op.sem = self.dma_sems[slot]
        op.val = self.dma_cnt[slot]
        inst.then_inc(op.sem, 16)
        self.dma_last[slot] = op
        self._record(op, q, reads, writes)
        if is_out:
            self.out_dmas.append(op)
        return op

    def barrier(self):
        assert not self.pending_pe
        lasts = [o for k, o in self.last_op.items() if not (isinstance(k, tuple))]
        dmas = [o for o in self.dma_last if o is not None]
        for eng in ("pe", "act", "dve", "pool", "sp"):
            for d in lasts + dmas:
                if d.is_dma or d.eng != eng:
                    self._emit_wait(eng, d)

    def finish(self):
        for d in self.dma_last:
            if d is not None:
                self._emit_wait("sp", d)


def _consts():
    bf = ml_dtypes.bfloat16
    j = np.arange(128)[:, None]
    s = np.arange(128)[None, :]
    c = {}
    c["ident_bf"] = np.eye(128, dtype=np.float32).astype(bf)
    c["ident_f"] = np.eye(128, dtype=np.float32)
    c["tri_neg"] = np.where(j >= s, -1.0, 0.0).astype(bf)
    c["ones_neg"] = np.full((128, 128), -1.0, np.float32).astype(bf)
    c["m01_strict"] = np.where(j < s, 1.0, 0.0).astype(bf)
    c["nm_strict"] = np.where(j < s, 0.0, -BIG).astype(bf)
    c["nm_causal"] = np.where(j <= s, 0.0, -BIG).astype(bf)
    c["pos"] = np.arange(S_LEN, dtype=np.float32)[None, :]
    inv64 = (10000.0 ** (-np.arange(0, 64, 2, dtype=np.float32) / 64.0)).astype(np.float32)
    c["inv_att"] = np.tile(inv64, 4)[:, None].astype(np.float32)
    inv256 = (10000.0 ** (-np.arange(0, 256, 2, dtype=np.float32) / 256.0)).astype(np.float32)
    c["inv_ret"] = inv256[:, None].astype(np.float32)
    kaug = np.zeros((32, S_LEN), np.float32)
    for n in range(16):
        kaug[1 + n, n * 256:(n + 1) * 256] = 1.0
    c["kaug"] = kaug.astype(bf)
    hh = np.arange(4, dtype=np.float64)
    g = 1.0 - 2.0 ** (-5.0 - hh)
    n = np.arange(128, dtype=np.float64)
    dm = np.zeros((4, 128, 128), np.float32)
    zk = np.zeros((128, 4), np.float32)
    xi = np.zeros((128, 4), np.float32)
    for h in range(4):
        z = g[h] ** (-(n + 1.0)) / 16.0
        zk[:, h] = z
        xi[:, h] = g[h] ** (n + 1.0)
        dm[h] = np.where(j <= s, z[:, None], 0.0)
    c["ret_dm"] = dm
    cc = np.arange(512, dtype=np.float64)[None, :]
    ss_ = np.arange(128, dtype=np.float64)[:, None]
    gf = np.zeros((4, 128, 512), np.float32)
    gm = np.zeros((4, 128, 512), np.float32)
    for h in range(4):
        full = g[h] ** (cc - ss_) / 16.0
        gf[h] = full
        gm[h] = np.where(cc >= ss_, full, 0.0)
    c["ret_gf"] = gf
    c["ret_gm"] = gm
    c["ret_zk"] = zk
    c["ret_xi"] = xi
    c["ret_cd"] = (g ** 128.0).astype(np.float32)
    return c


_CONST_SPECS = [
    ("ident_bf", [128, 128], BF16), ("ident_f", [128, 128], F32), ("tri_neg", [128, 128], BF16),
    ("ones_neg", [128, 128], BF16), ("m01_strict", [128, 128], BF16), ("nm_strict", [128, 128], BF16),
    ("nm_causal", [128, 128], BF16), ("pos", [1, S_LEN], F32), ("inv_att", [128, 1], F32),
    ("inv_ret", [128, 1], F32), ("kaug", [32, S_LEN], BF16), ("ret_dm", [4, 128, 128], F32),
    ("ret_zk", [128, 4], F32), ("ret_xi", [128, 4], F32), ("ret_gf", [4, 128, 512], F32), ("ret_gm", [4, 128, 512], F32),
]

_IN_SPECS = [
    ("x", [S_LEN, D]), ("c", [128, 8]), ("ada_w", [2, D, 6 * D]), ("ada_b", [2, 6 * D]),
    ("norm_gains", [2, 2, D]), ("att_w_qkv", [D, 3 * D]), ("att_w_o", [D, D]), ("ret_w_in", [D, 6 * D]),
    ("ret_gn", [1, 2 * D]), ("ret_w_o", [2 * D, D]), ("ffn_w_gate_up", [2, D, 2 * DFF]),
    ("ffn_w_down", [2, DFF, D]), ("final_norm", [1, D]),
]


def build_program(stop_after=None, debug=False):
    nc = bass.Bass("TRN2", target_bir_lowering=False)
    I = {}
    for name, shape in _IN_SPECS:
        I[name] = nc.dram_tensor(name, shape, F32, kind="ExternalInput").ap()
    C = {}
    for name, shape, dt in _CONST_SPECS:
        C[name] = nc.dram_tensor("k_" + name, shape, dt, kind="ExternalInput").ap()
    y_out = nc.dram_tensor("y", [S_LEN, D], F32, kind="ExternalOutput").ap()
    xs = nc.dram_tensor("xs", [S_LEN, D], F32).ap()
    oT_d = nc.dram_tensor("oT_d", [D, S_LEN], BF16).ap()
    uT_d = nc.dram_tensor("uT_d", [2 * D, S_LEN], BF16).ap()
    dbg = {}
    if debug:
        dbg["mod"] = nc.dram_tensor("dbg_mod", [128, 6 * D], F32, kind="ExternalOutput").ap()
        if stop_after != "ada":
            dbg["hT"] = nc.dram_tensor("dbg_hT", [128, 8 * S_LEN], BF16, kind="ExternalOutput").ap()
        if stop_after in ("sb0", "moba0", "att", "l0"):
            dbg["oT"] = nc.dram_tensor("dbg_oT", [D, S_LEN], BF16, kind="ExternalOutput").ap()

    _uid = [0]

    def sbt(name, shape, dt):
        _uid[0] += 1
        return nc.sbuf_tensor("%s_u%d" % (name, _uid[0]), shape, dt)

    with contextlib.ExitStack() as es:
        S = Sched(nc, es)
        sb = lambda name, shape, dt: es.enter_context(sbt(name, shape, dt))
        PS = [es.enter_context(nc.psum_tensor("ps%d" % i, [128, 512], F32)) for i in range(8)]
        PB = [Buf("ps%d" % i) for i in range(8)]

        ident_bf = sb("ident_bf", [128, 128], BF16)
        ident_f = sb("ident_f", [128, 128], F32)
        tri_neg = sb("tri_neg", [128, 128], BF16)
        ones_neg = sb("ones_neg", [128, 128], BF16)
        m01 = sb("m01", [128, 128], BF16)
        nm_strict = sb("nm_strict", [128, 128], BF16)
        nm_causal = sb("nm_causal", [128, 128], BF16)
        ones_f = sb("ones_f", [128, 128], F32)
        negpi = sb("negpi", [128, 1], F32)
        cb = Buf("consts")
        for t, nme in ((ident_bf, "ident_bf"), (ident_f, "ident_f"), (tri_neg, "tri_neg"), (ones_neg, "ones_neg"),
                       (m01, "m01_strict"), (nm_strict, "nm_strict"), (nm_causal, "nm_causal")):
            S.dma("sp", lambda t=t, nme=nme: nc.sync.dma_start(out=t[:], in_=C[nme][:, :]), [], [cb])
        S.dve(lambda: nc.vector.memset(ones_f[:], 1.0), [], [cb])
        S.dve(lambda: nc.vector.memset(negpi[:], -PI), [], [cb])
        zeros_bf = sb("zeros_bf", [128, 512], BF16)
        S.dve(lambda: nc.vector.memset(zeros_bf[:], 0.0), [], [cb])
        eps6 = sb("eps6", [128, 1], F32)
        eps5 = sb("eps5", [128, 1], F32)
        S.dve(lambda: nc.vector.memset(eps6[:], 1e-6), [], [cb])
        S.dve(lambda: nc.vector.memset(eps5[:], 1e-5), [], [cb])

        MOD = [sb("mod%d" % i, [128, D], F32) for i in range(6)]
        modb = [Buf("mod%d" % i) for i in range(6)]

        def ada_gen(layer, ph):
            psb = lambda name, shape, dt: ph.enter_context(sbt(name, shape, dt))
            ccol = psb("ccol", [128, 8], F32)
            scol = psb("scol", [128, 8], F32)
            rep = psb("rep", [128, 8, 128], F32)
            wch = [psb("adaw%d" % i, [128, 8, 512], F32) for i in range(2)]
            bch = [psb("adab%d" % i, [128, 512], F32) for i in range(2)]
            gt = [psb("adag%d" % i, [128, D], F32) for i in range(2)]
            b_c, b_rep = Buf(), Buf()
            b_w = [Buf(), Buf()]
            b_b = [Buf(), Buf()]
            b_g = [Buf(), Buf()]
            S.dma("sp", lambda: nc.sync.dma_start(out=ccol[:], in_=I["c"][:, :]), [], [b_c])
            S.act(lambda: nc.scalar.activation(out=scol[:], in_=ccol[:], func=AF.Silu), [b_c], [b_c])
            for k in range(8):
                S.dve(lambda k=k: nc.vector.tensor_scalar(out=rep[:, k, :], in0=ones_f[:], scalar1=scol[:, k:k + 1],
                                                          scalar2=None, op0=ALU.mult), [b_c, cb], [b_rep])
            for gi in range(2):
                S.dma("sp", lambda gi=gi: nc.sync.dma_start(
                    out=gt[gi][:], in_=I["norm_gains"][layer, gi:gi + 1, :].partition_broadcast(128)), [], [b_g[gi]])
            wv = I["ada_w"][layer].rearrange("(p k) n -> p k n", k=8)
            for ci in range(12):
                w_, bb_ = wch[ci % 2], bch[ci % 2]
                S.dma("sp", lambda ci=ci, w_=w_: nc.sync.dma_start(out=w_[:], in_=wv[:, :, ci * 512:(ci + 1) * 512]),
                      [], [b_w[ci % 2]])
                S.dma("sp", lambda ci=ci, bb_=bb_: nc.sync.dma_start(
                    out=bb_[:], in_=I["ada_b"][layer:layer + 1, ci * 512:(ci + 1) * 512].partition_broadcast(128)),
                    [], [b_b[ci % 2]])
                pb = ci % 2
                for k in range(8):
                    S.pe(lambda k=k, w_=w_, pb=pb: nc.tensor.matmul(PS[pb][:, :], lhsT=rep[:, k, :], rhs=w_[:, k, :],
                                                                    start=(k == 0), stop=(k == 7)),
                         [b_rep, b_w[ci % 2]], [PB[pb]], sig=(k == 7))
                jm, half = ci // 2, ci % 2
                S.dve(lambda jm=jm, half=half, pb=pb, bb_=bb_: nc.vector.tensor_tensor(
                    out=MOD[jm][:, half * 512:(half + 1) * 512], in0=PS[pb][:, :], in1=bb_[:], op=ALU.add),
                    [PB[pb], b_b[ci % 2]], [modb[jm]])
                for (js, gi, cdone) in ((1, 0, 3), (4, 1, 9)):
                    if ci == cdone:
                        S.dve(lambda js=js, gi=gi: nc.vector.scalar_tensor_tensor(
                            out=MOD[js][:], in0=MOD[js][:], scalar=1.0, in1=gt[gi][:], op0=ALU.add, op1=ALU.mult),
                            [modb[js], b_g[gi]], [modb[js]])
                yield

        A_M, B_M, G_M, A_F, B_F, G_F = 1, 0, 2, 4, 3, 5

        def norm_mod_transpose(x_src_ap, xbuf_token, a_idx, b_idx, hT, hT_buf, tt_glob_cols, tiles, wk):
            for tt in tiles:
                xt = x_src_ap(tt)
                xb = xbuf_token(tt)
                i2 = tt % 2
                ss, rstd, junk, tmp, hb = wk["ss"][i2], wk["rstd"][i2], wk["junk"], wk["tmp"][i2], wk["hb"][i2]
                b_ss, b_junk, b_tmp, b_hb = wk["b_ss"][i2], wk["b_junk"], wk["b_tmp"][i2], wk["b_hb"][i2]
                S.dve(lambda ss=ss: nc.vector.memset(ss[:], 0.0), [], [b_ss])
                S.act(lambda xt=xt, ss=ss, junk=junk: nc.scalar.activation(out=junk[:], in_=xt, func=AF.Square,
                                                                            accum_out=ss[:, 0:1]), [xb], [b_junk, b_ss])
                S.act(lambda ss=ss, rstd=rstd: nc.scalar.activation(out=rstd[:], in_=ss[:], func=AF.Sqrt, bias=eps6[:, 0:1],
                                                                    scale=1.0 / D), [b_ss, cb], [b_ss])
                S.dve(lambda rstd=rstd: nc.vector.reciprocal(out=rstd[:], in_=rstd[:]), [b_ss], [b_ss])
                S.dve(lambda xt=xt, rstd=rstd, tmp=tmp: nc.vector.scalar_tensor_tensor(
                    out=tmp[:], in0=xt, scalar=rstd[:, 0:1], in1=MOD[a_idx][:], op0=ALU.mult, op1=ALU.mult),
                    [xb, b_ss, modb[a_idx]], [b_tmp])
                S.pool(lambda tmp=tmp, hb=hb: nc.gpsimd.tensor_tensor(out=hb[:], in0=tmp[:], in1=MOD[b_idx][:], op=ALU.add),
                       [b_tmp, modb[b_idx]], [b_hb])
                pb = wk["psT"][i2]
                pst = PS[pb].bitcast(BF16)
                for k in range(8):
                    S.pe(lambda k=k, hb=hb, pst=pst: nc.tensor.transpose(out=pst[:, k * 128:(k + 1) * 128],
                                                                        in_=hb[:, k * 128:(k + 1) * 128], identity=ident_bf[:]),
                         [b_hb, cb], [PB[pb]], sig=(k == 7))
                c0 = tt_glob_cols(tt)
                S.act(lambda pst=pst, c0=c0: nc.scalar.activation(
                    out=hT[:, :, c0:c0 + 128], in_=pst.rearrange("p (k t) -> p k t", k=8), func=AF.Copy),
                    [PB[pb]], [hT_buf(tt)])

        def make_norm_wk(ph, pfx, psT=(6, 7), single=False):
            psb = lambda name, shape, dt: ph.enter_context(sbt(pfx + name, shape, dt))
            if single:
                tmp_ = psb("tmp", [128, D], F32)
                hb_ = psb("hb", [128, D], BF16)
                bt, bh = Buf(), Buf()
                return {
                    "ss": [psb("ss%d" % i, [128, 1], F32) for i in range(2)],
                    "rstd": [psb("rstd%d" % i, [128, 1], F32) for i in range(2)],
                    "junk": psb("junk", [128, D], BF16), "tmp": [tmp_, tmp_], "hb": [hb_, hb_],
                    "b_ss": [Buf(), Buf()], "b_junk": Buf(), "b_tmp": [bt, bt], "b_hb": [bh, bh], "psT": psT,
                }
            return {
                "ss": [psb("ss%d" % i, [128, 1], F32) for i in range(2)],
                "rstd": [psb("rstd%d" % i, [128, 1], F32) for i in range(2)],
                "junk": psb("junk", [128, D], BF16),
                "tmp": [psb("tmp%d" % i, [128, D], F32) for i in range(2)],
                "hb": [psb("hb%d" % i, [128, D], BF16) for i in range(2)],
                "b_ss": [Buf(), Buf()], "b_junk": Buf(), "b_tmp": [Buf(), Buf()], "b_hb": [Buf(), Buf()],
                "psT": psT,
            }

        def rope_tables(ph, pfx, inv_name, cosT, sinT, b_tab):
            with contextlib.ExitStack() as tp:
                psb = lambda name, shape, dt: tp.enter_context(sbt(pfx + name, shape, dt))
                inv = psb("inv", [128, 1], F32)
                ang = psb("ang", [128, 1024], F32)
                yy = psb("yy", [128, 1024], F32)
                ni = psb("ni", [128, 1024], mybir.dt.int32)
                b_inv, b_ang, b_y, b_n = Buf(), Buf(), Buf(), Buf()
                S.dma("sp", lambda: nc.sync.dma_start(out=inv[:], in_=C[inv_name][:, :]), [], [b_inv])
                for ch in range(4):
                    cs = slice(ch * 1024, (ch + 1) * 1024)
                    S.dma("sp", lambda cs=cs: nc.sync.dma_start(out=ang[:], in_=C["pos"][0:1, cs].partition_broadcast(128)),
                          [], [b_ang])
                    S.dve(lambda: nc.vector.tensor_scalar(out=ang[:], in0=ang[:], scalar1=inv[:, 0:1], scalar2=None,
                                                          op0=ALU.mult), [b_inv, b_ang], [b_ang])
                    for (dst, off) in ((sinT, 0.0), (cosT, 0.5 * PI)):
                        S.dve(lambda off=off: nc.vector.tensor_scalar(out=yy[:], in0=ang[:], scalar1=1.0 / (2 * PI),
                                                                      scalar2=off / (2 * PI) + 0.5, op0=ALU.mult, op1=ALU.add),
                              [b_ang], [b_y])
                        S.dve(lambda: nc.vector.tensor_copy(out=ni[:], in_=yy[:]), [b_y], [b_n])
                        S.dve(lambda: nc.vector.tensor_copy(out=yy[:], in_=ni[:]), [b_n], [b_y])
                        S.dve(lambda: nc.vector.scalar_tensor_tensor(out=yy[:], in0=yy[:], scalar=-2 * PI, in1=ang[:],
                                                                     op0=ALU.mult, op1=ALU.add), [b_y, b_ang], [b_y])
                        if off != 0.0:
                            S.dve(lambda off=off: nc.vector.tensor_scalar(out=yy[:], in0=yy[:], scalar1=off, scalar2=None,
                                                                          op0=ALU.add), [b_y], [b_y])
                        S.dve(lambda cs=cs, dst=dst: nc.vector.tensor_scalar(out=dst[:, cs], in0=yy[:], scalar1=-PI,
                                                                             scalar2=2 * PI, op0=ALU.is_lt, op1=ALU.mult),
                              [b_y], [b_tab])
                        S.dve(lambda cs=cs, dst=dst: nc.vector.tensor_tensor(out=yy[:], in0=yy[:], in1=dst[:, cs], op=ALU.add),
                              [b_y, b_tab], [b_y])
                        S.dve(lambda cs=cs, dst=dst: nc.vector.tensor_scalar(out=dst[:, cs], in0=yy[:], scalar1=PI,
                                                                             scalar2=-2 * PI, op0=ALU.is_gt, op1=ALU.mult),
                              [b_y], [b_tab])
                        S.dve(lambda cs=cs, dst=dst: nc.vector.tensor_tensor(out=yy[:], in0=yy[:], in1=dst[:, cs], op=ALU.add),
                              [b_y, b_tab], [b_y])
                        S.act(lambda cs=cs, dst=dst: nc.scalar.activation(out=dst[:, cs], in_=yy[:], func=AF.Sin),
                              [b_y], [b_tab])
                S.barrier()

        def phase_att():
            with contextlib.ExitStack() as ph:
                psb = lambda name, shape, dt: ph.enter_context(sbt(name, shape, dt))
                hT = psb("hT", [128, 8, S_LEN], BF16)
                b_hT = [Buf("hT%d" % i) for i in range(NQC)]
                with contextlib.ExitStack() as ph1:
                    wk = make_norm_wk(ph1, "a_")
                    xt = [ph1.enter_context(sbt("a_xt%d" % i, [128, D], F32)) for i in range(2)]
                    b_xt = [Buf(), Buf()]
                    ag = ada_gen(0, ph1)
                    for _ in range(4):
                        next(ag)
                    for tt in range(NT):
                        S.dma("sp", lambda tt=tt: nc.sync.dma_start(out=xt[tt % 2][:], in_=I["x"][tt * 128:(tt + 1) * 128, :]),
                              [], [b_xt[tt % 2]])
                        norm_mod_transpose(lambda t: xt[t % 2][:], lambda t: b_xt[t % 2], A_M, B_M, hT,
                                           lambda t: b_hT[t // 4], lambda t: t * 128, [tt], wk)
                        if tt % 4 == 3:
                            next(ag, None)
                    for _ in ag:
                        pass
                    S.barrier()
                if debug:
                    S.dma("sp", lambda: nc.sync.dma_start(out=dbg["hT"][:, :], in_=hT[:].rearrange("p k t -> p (k t)")),
                          b_hT, [], is_out=True)
                if stop_after == "hT":
                    return
                cosT = psb("cosT", [128, S_LEN], F32)
                sinT = psb("sinT", [128, S_LEN], F32)
                b_tab = Buf("tab")
                rope_tables(ph, "a_", "inv_att", cosT, sinT, b_tab)

                wq = psb("wq", [128, 8, 128], BF16)
                wkk = psb("wk", [128, 8, 128], BF16)
                wv = psb("wv", [128, 8, 128], BF16)
                wqs = psb("wqs", [128, 8, 128], BF16)
                wks = psb("wks", [128, 8, 128], BF16)
                b_w = Buf("w")
                b_ws = Buf("ws")
                qT = [psb("qT%d" % i, [96, S_LEN], BF16) for i in range(2)]
                kT = [psb("kT%d" % i, [96, S_LEN], BF16) for i in range(2)]
                vv = psb("vv", [128, NT, 2, 65], BF16)
                b_q = [Buf(), Buf()]
                b_k = [Buf(), Buf()]
                b_v = Buf()
                t1 = psb("t1", [128, 512], F32)
                t2 = psb("t2", [128, 512], F32)
                t3 = t1
                b_t1, b_t2 = Buf(), Buf()
                b_t3 = b_t1
                e_sb = [[psb("e_sb%d_%d" % (h_, i), [128, 512], BF16) for i in range(2)] for h_ in range(2)]
                sp_sb = [[psb("sp_sb%d_%d" % (h_, i), [128, 512], BF16) for i in range(2)] for h_ in range(2)]
                a_sb = [[psb("a_sb%d_%d" % (h_, i), [128, 512], BF16) for i in range(2)] for h_ in range(2)]
                ssum = [[psb("ssum%d_%d" % (h_, i), [128, 512], BF16) for i in range(2)] for h_ in range(2)]
                o_sb = [[psb("o_sb%d_%d" % (h_, i), [64, 512], BF16) for i in range(2)] for h_ in range(2)]
                b_e = [[Buf(), Buf()] for _ in range(2)]
                b_sp = [[Buf(), Buf()] for _ in range(2)]
                b_a = [[Buf(), Buf()] for _ in range(2)]
                b_ssum = [[Buf(), Buf()] for _ in range(2)]
                b_o = [[Buf(), Buf()] for _ in range(2)]
                b_oT = [[Buf() for _ in range(NQC)] for _ in range(16)]
                kmean = psb("kmean", [64, 16], F32)
                kmean_hi = psb("kmean_hi", [64, 16], BF16)
                kmean_lo = psb("kmean_lo", [64, 16], BF16)
                kmr = psb("kmr", [64, 16], F32)
                b_km = Buf()
                gpad = [psb("gpad%d" % i, [128, 16], F32) for i in range(2)]
                top8 = [psb("top8%d" % i, [128, 8], F32) for i in range(2)]
                xaug = [psb("xaug%d" % i, [128, 32], F32) for i in range(2)]
                b_g = [Buf(), Buf()]
                rden = [psb("rden%d" % i, [1, 512], F32) for i in range(2)]
                of_sb = [psb("of_sb%d" % i, [64, 512], F32) for i in range(2)]
                b_rden, b_of = [Buf(), Buf()], [Buf(), Buf()]
                ones_row = psb("ones_row", [65, 64], F32)
                S.dve(lambda: nc.vector.memset(ones_row[:], 1.0), [], [cb])
                S.dve(lambda: nc.vector.memset(vv[:], 1.0), [], [b_v])
                for i in range(2):
                    S.dma("sp", lambda i=i: nc.sync.dma_start(out=kT[i][64:96, :], in_=C["kaug"][:, :]), [], [b_k[i]])

                wsrc = I["att_w_qkv"].rearrange("(k p) n -> p k n", p=128)

                def project_pair(hp, moba):
                    c0 = hp * 128
                    S.dma("pool", lambda: nc.gpsimd.dma_start(out=wq[:], in_=wsrc[:, :, c0:c0 + 128]), [], [b_w])
                    S.dma("pool", lambda: nc.gpsimd.dma_start(out=wkk[:], in_=wsrc[:, :, D + c0:D + c0 + 128]), [], [b_w])
                    S.dma("pool", lambda: nc.gpsimd.dma_start(out=wv[:], in_=wsrc[:, :, 2 * D + c0:2 * D + c0 + 128]), [], [b_w])
                    if moba:
                        for (src, dst) in ((wq, wqs), (wkk, wks)):
                            for hh in range(2):
                                b0 = hh * 64
                                S.act(lambda src=src, dst=dst, b0=b0: nc.scalar.mul(out=dst[:, :, b0:b0 + 32],
                                                                                    in_=src[:, :, b0 + 32:b0 + 64], mul=-1.0),
                                      [b_w], [b_ws])
                                S.act(lambda src=src, dst=dst, b0=b0: nc.scalar.copy(out=dst[:, :, b0 + 32:b0 + 64],
                                                                                     in_=src[:, :, b0:b0 + 32]),
                                      [b_w], [b_ws])
                    for (w_, ws_, dst, bdst, scale) in ((wq, wqs, qT, b_q, 0.125), (wkk, wks, kT, b_k, 1.0)):
                        for tc in range(NQC):
                            cols = slice(tc * 512, (tc + 1) * 512)
                            pa = tc % 2
                            for k in range(8):
                                S.pe(lambda k=k, w_=w_, pa=pa, cols=cols: nc.tensor.matmul(
                                    PS[pa][:, :], lhsT=w_[:, k, :], rhs=hT[:, k, cols], start=(k == 0), stop=(k == 7)),
                                    [b_w, b_hT[tc]], [PB[pa]], sig=(k == 7))
                            if not moba:
                                for hh in range(2):
                                    S.act(lambda hh=hh, pa=pa, cols=cols, dst=dst, scale=scale: nc.scalar.mul(
                                        out=dst[hh][0:64, cols], in_=PS[pa][hh * 64:(hh + 1) * 64, :], mul=scale),
                                        [PB[pa]], [bdst[hh]])
                            else:
                                pb2 = 2 + tc % 2
                                for k in range(8):
                                    S.pe(lambda k=k, ws_=ws_, pb2=pb2, cols=cols: nc.tensor.matmul(
                                        PS[pb2][:, :], lhsT=ws_[:, k, :], rhs=hT[:, k, cols], start=(k == 0), stop=(k == 7)),
                                        [b_ws, b_hT[tc]], [PB[pb2]], sig=(k == 7))
                                S.dve(lambda pa=pa, cols=cols: nc.vector.tensor_tensor(out=t1[:], in0=PS[pa][:, :],
                                                                                       in1=cosT[:, cols], op=ALU.mult),
                                      [PB[pa], b_tab], [b_t1])
                                S.dve(lambda pb2=pb2, cols=cols: nc.vector.tensor_tensor(out=t2[:], in0=PS[pb2][:, :],
                                                                                         in1=sinT[:, cols], op=ALU.mult),
                                      [PB[pb2], b_tab], [b_t2])
                                S.pool(lambda: nc.gpsimd.tensor_tensor(out=t3[:], in0=t1[:], in1=t2[:], op=ALU.add),
                                       [b_t1, b_t2], [b_t3])
                                for hh in range(2):
                                    S.act(lambda hh=hh, cols=cols, dst=dst, scale=scale: nc.scalar.mul(
                                        out=dst[hh][0:64, cols], in_=t3[hh * 64:(hh + 1) * 64, :], mul=scale),
                                        [b_t3], [bdst[hh]])
                    for g4 in range(NT // 4):
                        pa = 4 + g4 % 2
                        for j in range(4):
                            tt = g4 * 4 + j
                            for k in range(8):
                                S.pe(lambda k=k, tt=tt, j=j, pa=pa: nc.tensor.matmul(
                                    PS[pa][:, j * 128:(j + 1) * 128], lhsT=hT[:, k, tt * 128:(tt + 1) * 128], rhs=wv[:, k, :],
                                    start=(k == 0), stop=(k == 7)), [b_w, b_hT[tt // 4]], [PB[pa]], sig=(j == 3 and k == 7))
                        S.act(lambda g4=g4, pa=pa: nc.scalar.activation(
                            out=vv[:, g4 * 4:(g4 + 1) * 4, :, 0:64],
                            in_=PS[pa][:, :].rearrange("p (j h d) -> p j h d", j=4, h=2), func=AF.Copy),
                            [PB[pa]], [b_v])

                def sb_head(hh, head):
                    q_, k_ = qT[hh], kT[hh]
                    pO = 4 + hh
                    e_, sp_, a_, ss_, o_ = e_sb[hh], sp_sb[hh], a_sb[hh], ssum[hh], o_sb[hh]
                    be_, bsp_, ba_, bss_, bo_ = b_e[hh], b_sp[hh], b_a[hh], b_ssum[hh], b_o[hh]
                    for qc in range(NQC):
                        kts = list(range(4 * qc + 3, -1, -1))
                        n_t = len(kts)

                        def par(idx):
                            kt = kts[idx]
                            j = kt - 4 * qc
                            return kt, j, (128 * j if j >= 0 else 0)

                        def st1(idx):
                            kt, j, c_lo = par(idx)
                            lc = slice(c_lo, 512)
                            tri = slice(c_lo, c_lo + 128)
                            qcols = slice(qc * 512 + c_lo, (qc + 1) * 512)
                            kcols = slice(kt * 128, (kt + 1) * 128)
                            i2 = idx % 2
                            pz = 2 * hh + i2
                            S.pe(lambda: nc.tensor.matmul(PS[pz][:, lc], lhsT=k_[0:64, kcols], rhs=q_[0:64, qcols],
                                                          start=True, stop=False), [b_k[hh], b_q[hh]], [PB[pz]], sig=True)
                            S.act(lambda: nc.scalar.activation(out=e_[i2][:, lc], in_=PS[pz][:, lc], func=AF.Exp),
                                  [PB[pz]], [be_[i2]])
                            S.act(lambda: nc.scalar.activation(out=sp_[i2][:, lc], in_=e_[i2][:, lc], func=AF.Ln,
                                                               bias=ones_f[:, 0:1], scale=1.0), [be_[i2], cb], [bsp_[i2]])
                            if j >= 0:
                                S.pool(lambda: nc.gpsimd.tensor_tensor(out=sp_[i2][:, tri], in0=sp_[i2][:, tri], in1=m01[:],
                                                                       op=ALU.mult), [bsp_[i2], cb], [bsp_[i2]])
                            if idx + 1 < n_t:
                                nb_ = (idx + 1) % 2
                                if idx == 0:
                                    S.dve(lambda: nc.vector.tensor_copy(out=ss_[nb_][:, lc], in_=sp_[i2][:, lc]),
                                          [bsp_[i2]], [bss_[nb_]])
                                else:
                                    S.dve(lambda: nc.vector.tensor_tensor(out=ss_[nb_][:, lc], in0=ss_[idx % 2][:, lc],
                                                                          in1=sp_[i2][:, lc], op=ALU.add),
                                          [bsp_[i2], bss_[idx % 2]], [bss_[nb_]])
                                c_lo2 = par(idx + 1)[2]
                                if c_lo2 < c_lo:
                                    S.dve(lambda: nc.vector.memset(ss_[nb_][:, c_lo2:c_lo], 0.0), [], [bss_[nb_]])

                        def st2(idx):
                            kt, j, c_lo = par(idx)
                            lc = slice(c_lo, 512)
                            tri = slice(c_lo, c_lo + 128)
                            i2 = idx % 2
                            pB = 2 * hh + i2
                            first = idx == 0
                            S.pe(lambda: nc.tensor.matmul(PS[pB][:, lc], lhsT=tri_neg[:], rhs=sp_[i2][:, lc], start=False,
                                                          stop=False), [cb, bsp_[i2]], [PB[pB]])
                            if not first:
                                S.pe(lambda: nc.tensor.matmul(PS[pB][:, lc], lhsT=ones_neg[:], rhs=ss_[idx % 2][:, lc],
                                                              start=False, stop=(j < 0)), [cb, bss_[idx % 2]], [PB[pB]],
                                     sig=(j < 0))
                            if j >= 0:
                                S.pe(lambda: nc.tensor.matmul(PS[pB][:, tri], lhsT=ident_bf[:], rhs=nm_strict[:], start=False,
                                                              stop=True), [cb], [PB[pB]], sig=True)
                            S.act(lambda: nc.scalar.activation(out=a_[i2][:, lc], in_=PS[pB][:, lc], func=AF.Exp),
                                  [PB[pB]], [ba_[i2]])

                        def st3(idx):
                            kt, j, c_lo = par(idx)
                            lc = slice(c_lo, 512)
                            i2 = idx % 2
                            if idx == 0:
                                S.pe(lambda: nc.tensor.matmul(PS[pO][0:64, :], lhsT=zeros_bf[:, 0:64], rhs=zeros_bf[:, :],
                                                              start=True, stop=False), [cb], [PB[pO]])
                            S.pe(lambda: nc.tensor.matmul(PS[pO][0:64, lc], lhsT=vv[:, kt, hh, 0:64], rhs=a_[i2][:, lc],
                                                          start=False, stop=(idx == n_t - 1)), [b_v, ba_[i2]], [PB[pO]], sig=True)

                        for stp in range(n_t + 2):
                            if 0 <= stp - 2 < n_t:
                                st3(stp - 2)
                            if 0 <= stp - 1 < n_t:
                                st2(stp - 1)
                            if stp < n_t:
                                st1(stp)
                            yield
                        ob = qc % 2
                        S.act(lambda ob=ob: nc.scalar.activation(out=o_[ob][:, :], in_=PS[pO][0:64, :], func=AF.Copy),
                              [PB[pO]], [bo_[ob]])
                        S.dma("sp", lambda ob=ob, qc=qc: nc.sync.dma_start(
                            out=oT_d[head * 64:(head + 1) * 64, qc * 512:(qc + 1) * 512], in_=o_[ob][:, :]),
                            [bo_[ob]], [b_oT[head][qc]])
                        yield

                def moba_gate(hh, head):
                    q_, k_ = qT[hh], kT[hh]
                    S.dve(lambda: nc.vector.tensor_reduce(out=kmean[:], in_=k_[0:64, :].rearrange("p (n k) -> p n k", k=256),
                                                          axis=AX.X, op=ALU.add), [b_k[hh]], [b_km])
                    S.act(lambda: nc.scalar.copy(out=kmean_hi[:], in_=kmean[:]), [b_km], [b_km])
                    S.dve(lambda: nc.vector.tensor_tensor(out=kmr[:], in0=kmean[:], in1=kmean_hi[:], op=ALU.subtract),
                          [b_km], [b_km])
                    S.act(lambda: nc.scalar.copy(out=kmean_lo[:], in_=kmr[:]), [b_km], [b_km])
                    for tt in range(NT):
                        own = tt // 2
                        i2 = tt % 2
                        pg = i2
                        tcols = slice(tt * 128, (tt + 1) * 128)
                        S.dve(lambda i2=i2: nc.vector.memset(xaug[i2][:], 0.0), [], [b_g[i2]])
                        if own >= 4:
                            S.pe(lambda pg=pg, tcols=tcols: nc.tensor.matmul(PS[pg][:, 0:16], lhsT=q_[0:64, tcols],
                                                                            rhs=kmean_hi[:], start=True, stop=False),
                                 [b_q[hh], b_km], [PB[pg]])
                            S.pe(lambda pg=pg, tcols=tcols: nc.tensor.matmul(PS[pg][:, 0:16], lhsT=q_[0:64, tcols],
                                                                            rhs=kmean_lo[:], start=False, stop=True),
                                 [b_q[hh], b_km], [PB[pg]], sig=True)
                            S.dve(lambda i2=i2: nc.vector.memset(gpad[i2][:], -1e30), [], [b_g[i2]])
                            S.dve(lambda i2=i2, pg=pg, own=own: nc.vector.tensor_copy(out=gpad[i2][:, 0:own],
                                                                                      in_=PS[pg][:, 0:own]),
                                  [PB[pg]], [b_g[i2]])
                            S.dve(lambda i2=i2: nc.vector.max(out=top8[i2][:], in_=gpad[i2][:]), [b_g[i2]], [b_g[i2]])
                            S.dve(lambda i2=i2, own=own: nc.vector.tensor_scalar(
                                out=xaug[i2][:, 1:1 + own], in0=gpad[i2][:, 0:own], scalar1=top8[i2][:, 2:3], scalar2=None,
                                op0=ALU.is_ge), [b_g[i2]], [b_g[i2]])
                            S.dve(lambda i2=i2, own=own: nc.vector.tensor_scalar(
                                out=xaug[i2][:, 1:1 + own], in0=xaug[i2][:, 1:1 + own], scalar1=-1.0, scalar2=BIG,
                                op0=ALU.add, op1=ALU.mult), [b_g[i2]], [b_g[i2]])
                        pt = 2 + i2
                        S.pe(lambda pt=pt, i2=i2: nc.tensor.matmul(PS[pt][0:32, 0:128], lhsT=xaug[i2][:, :], rhs=ident_f[:],
                                                                   start=True, stop=True), [b_g[i2], cb], [PB[pt]], sig=True)
                        S.act(lambda pt=pt, tcols=tcols: nc.scalar.activation(out=q_[64:96, tcols], in_=PS[pt][0:32, 0:128],
                                                                              func=AF.Copy), [PB[pt]], [b_q[hh]])

                def moba_head(hh, head):
                    q_, k_ = qT[hh], kT[hh]
                    a_, o_, ba_, bo_ = a_sb[hh], o_sb[hh], b_a[hh], b_o[hh]
                    for qc in range(NQC):
                        pO = 4 + hh
                        nk = 4 * qc + 4

                        def m1(kt):
                            j = kt - 4 * qc
                            c_lo = 128 * j if j >= 0 else 0
                            qcols = slice(qc * 512 + c_lo, (qc + 1) * 512)
                            lc = slice(c_lo, 512)
                            tri = slice(c_lo, c_lo + 128)
                            kcols = slice(kt * 128, (kt + 1) * 128)
                            i2 = kt % 2
                            pz = 2 * hh + i2
                            S.pe(lambda: nc.tensor.matmul(PS[pz][:, lc], lhsT=k_[0:96, kcols], rhs=q_[0:96, qcols], start=True,
                                                          stop=(j < 0)), [b_k[hh], b_q[hh]], [PB[pz]], sig=(j < 0))
                            if j >= 0:
                                S.pe(lambda: nc.tensor.matmul(PS[pz][:, tri], lhsT=ident_bf[:], rhs=nm_causal[:], start=False,
                                                              stop=True), [cb], [PB[pz]], sig=True)
                            S.act(lambda: nc.scalar.activation(out=a_[i2][:, lc], in_=PS[pz][:, lc], func=AF.Exp),
                                  [PB[pz]], [ba_[i2]])

                        def m2(kt):
                            j = kt - 4 * qc
                            c_lo = 128 * j if j >= 0 else 0
                            lc = slice(c_lo, 512)
                            i2 = kt % 2
                            S.pe(lambda: nc.tensor.matmul(PS[pO][0:65, lc], lhsT=vv[:, kt, hh, :], rhs=a_[i2][:, lc],
                                                          start=(kt == 0), stop=(kt == nk - 1)), [b_v, ba_[i2]], [PB[pO]], sig=True)

                        for stp in range(nk + 1):
                            if 0 <= stp - 1 < nk:
                                m2(stp - 1)
                            if stp < nk:
                                m1(stp)
                            yield
                        S.dve(lambda: nc.vector.reciprocal(out=rden[hh][0:1, :], in_=PS[pO][64:65, :]), [PB[pO]], [b_rden[hh]])
                        S.act(lambda: nc.scalar.copy(out=of_sb[hh][:, :], in_=PS[pO][0:64, :]), [PB[pO]], [b_of[hh]])
                        pb = 6 + hh
                        S.pe(lambda: nc.tensor.matmul(PS[pb][0:64, :], lhsT=ones_row[0:1, :], rhs=rden[hh][0:1, :],
                                                      start=True, stop=True), [cb, b_rden[hh]], [PB[pb]], sig=True)
                        ob = qc % 2
                        S.dve(lambda ob=ob: nc.vector.tensor_tensor(out=o_[ob][:, :], in0=of_sb[hh][:, :], in1=PS[pb][0:64, :],
                                                                    op=ALU.mult), [b_of[hh], PB[pb]], [bo_[ob]])
                        S.dma("sp", lambda ob=ob, qc=qc: nc.sync.dma_start(
                            out=oT_d[head * 64:(head + 1) * 64, qc * 512:(qc + 1) * 512], in_=o_[ob][:, :]),
                            [bo_[ob]], [b_oT[head][qc]])
                        yield

                pairs = list(range(8))
                if stop_after == "sb0":
                    pairs = [0]
                if stop_after == "moba0":
                    pairs = [4]
                for hp in pairs:
                    moba = hp >= 4
                    project_pair(hp, moba)
                    if moba:
                        for hh in range(2):
                            moba_gate(hh, hp * 2 + hh)
                        gens = [moba_head(hh, hp * 2 + hh) for hh in range(2)]
                    else:
                        gens = [sb_head(hh, hp * 2 + hh) for hh in range(2)]
                    while gens:
                        for g_ in list(gens):
                            try:
                                next(g_)
                            except StopIteration:
                                gens.remove(g_)
                if debug:
                    allo = [b for row in b_oT for b in row]
                    S.dma("sp", lambda: nc.sync.dma_start(out=dbg["oT"][:, :], in_=oT_d[:, :]), allo, [], is_out=True)
                S.barrier()

        b_xs = [Buf("xs%d" % i) for i in range(NT)]

        def phase_outproj(x_src, mixT_d, FC, w_src):
            with contextlib.ExitStack() as ph:
                psb = lambda name, shape, dt: ph.enter_context(sbt(name, shape, dt))
                wo = psb("wo", [128, FC, D], BF16)
                b_wo = Buf()
                S.dma("pool", lambda: nc.gpsimd.dma_start(out=wo[:], in_=w_src.rearrange("(c p) n -> p c n", p=128)), [], [b_wo])
                mix = [psb("mix%d" % i, [128, FC, 512], BF16) for i in range(2)]
                b_mix = [Buf(), Buf()]
                xt = [psb("o_xt%d" % i, [128, D], F32) for i in range(2)]
                tmp = [psb("o_tmp%d" % i, [128, D], F32) for i in range(2)]
                b_xt, b_tmp = [Buf(), Buf()], [Buf(), Buf()]
                mv = mixT_d.rearrange("(c p) t -> p c t", p=128)

                def load_mix(c):
                    S.dma("sp", lambda: nc.sync.dma_start(out=mix[c % 2][:], in_=mv[:, :, c * 512:(c + 1) * 512]),
                          [], [b_mix[c % 2]])

                def load_x(tt):
                    S.dma("sp", lambda: nc.sync.dma_start(out=xt[tt % 2][:], in_=x_src[tt * 128:(tt + 1) * 128, :]),
                          [b_xs[tt]], [b_xt[tt % 2]])

                load_mix(0)
                load_x(0)
                for c in range(NQC):
                    for j in range(4):
                        tt = c * 4 + j
                        i2 = tt % 2
                        for half in range(2):
                            pb = half + 2 * i2
                            hs = slice(half * 512, (half + 1) * 512)
                            for cc in range(FC):
                                S.pe(lambda cc=cc, c=c, j=j, pb=pb, hs=hs: nc.tensor.matmul(
                                    PS[pb][:, :], lhsT=mix[c % 2][:, cc, j * 128:(j + 1) * 128], rhs=wo[:, cc, hs],
                                    start=(cc == 0), stop=(cc == FC - 1)), [b_mix[c % 2], b_wo], [PB[pb]], sig=(cc == FC - 1))
                            S.dve(lambda i2=i2, pb=pb, hs=hs: nc.vector.tensor_tensor(out=tmp[i2][:, hs], in0=PS[pb][:, :],
                                                                                      in1=MOD[G_M][:, hs], op=ALU.mult),
                                  [PB[pb], modb[G_M]], [b_tmp[i2]])
                        S.pool(lambda i2=i2: nc.gpsimd.tensor_tensor(out=xt[i2][:], in0=tmp[i2][:], in1=xt[i2][:], op=ALU.add),
                               [b_tmp[i2], b_xt[i2]], [b_xt[i2]])
                        if tt + 1 < NT:
                            load_x(tt + 1)
                        if j == 0 and c + 1 < NQC:
                            load_mix(c + 1)
                        S.dma("sp", lambda tt=tt, i2=i2: nc.sync.dma_start(out=xs[tt * 128:(tt + 1) * 128, :], in_=xt[i2][:]),
                              [b_xt[i2]], [b_xs[tt]])
                S.barrier()

        def phase_ffn(layer, final):
            with contextlib.ExitStack() as ph:
                psb = lambda name, shape, dt: ph.enter_context(sbt(name, shape, dt))
                wgu = psb("wgu", [128, 8, 2 * DFF], BF16)
                wd = psb("wd", [128, NFC, D], BF16)
                b_wgu, b_wd = Buf(), Buf()
                gsrc = I["ffn_w_gate_up"][layer].rearrange("(k p) n -> p k n", p=128)
                fb = [0, 6, 12, 17, NFC]
                b_wgu_q = [Buf() for _ in range(4)]
                fq = lambda f: 0 if f < 6 else (1 if f < 12 else (2 if f < 17 else 3))
                for qi in range(4):
                    c0, c1 = fb[qi] * 128, fb[qi + 1] * 128
                    S.dma("pool", lambda c0=c0, c1=c1: nc.gpsimd.dma_start(out=wgu[:, :, c0:c1], in_=gsrc[:, :, c0:c1]),
                          [], [b_wgu_q[qi]])
                    S.dma("pool", lambda c0=c0, c1=c1: nc.gpsimd.dma_start(out=wgu[:, :, DFF + c0:DFF + c1],
                                                                           in_=gsrc[:, :, DFF + c0:DFF + c1]),
                          [], [b_wgu_q[qi]])
                dsrc = I["ffn_w_down"][layer].rearrange("(c p) n -> p c n", p=128)
                for f0 in range(0, NFC, 11):
                    S.dma("pool", lambda f0=f0: nc.gpsimd.dma_start(out=wd[:, f0:f0 + 11, :], in_=dsrc[:, f0:f0 + 11, :]),
                          [], [b_wd])
                wk = make_norm_wk(ph, "f_", psT=(4, 5), single=True)
                h2T = psb("h2T", [128, 8, 512], BF16)
                b_h2T = Buf()
                actT = psb("actT", [128, NFC, 512], BF16)
                b_act = [Buf() for _ in range(NFC)]
                sg0 = psb("sg0", [128, 512], F32)
                sg = [sg0, sg0]
                bsg0 = Buf()
                b_sg = [bsg0, bsg0]
                xt = [psb("f_xt%d" % i, [128, D], F32) for i in range(2)]
                b_xt = [Buf(), Buf()]
                tmp = wk["tmp"][0]
                b_tmp = wk["b_tmp"][0]
                if final:
                    fn = MOD[A_M]
                    b_fn = modb[A_M]
                    S.dma("sp", lambda: nc.sync.dma_start(out=fn[:], in_=I["final_norm"][0:1, :].partition_broadcast(128)),
                          [], [b_fn])
                    fss = psb("fss", [128, 1], F32)
                    frs = psb("frs", [128, 1], F32)
                    b_fs = Buf()
                for c in range(NQC):
                    for j in range(4):
                        tt = c * 4 + j
                        i2 = tt % 2
                        S.dma("sp", lambda tt=tt, i2=i2: nc.sync.dma_start(out=xt[i2][:], in_=xs[tt * 128:(tt + 1) * 128, :]),
                              [b_xs[tt]], [b_xt[i2]])
                        norm_mod_transpose(lambda t: xt[t % 2][:], lambda t: b_xt[t % 2], A_F, B_F, h2T,
                                           lambda t: b_h2T, lambda t: (t % 4) * 128, [tt], wk)
                    for f in range(NFC):
                        pg, pu = (0, 1) if f % 2 == 0 else (2, 3)
                        for k in range(8):
                            S.pe(lambda k=k, f=f, pg=pg: nc.tensor.matmul(PS[pg][:, :], lhsT=wgu[:, k, f * 128:(f + 1) * 128],
                                                                         rhs=h2T[:, k, :], start=(k == 0), stop=(k == 7)),
                                 [b_wgu_q[fq(f)], b_h2T], [PB[pg]], sig=(k == 7))
                        for k in range(8):
                            S.pe(lambda k=k, f=f, pu=pu: nc.tensor.matmul(
                                PS[pu][:, :], lhsT=wgu[:, k, DFF + f * 128:DFF + (f + 1) * 128], rhs=h2T[:, k, :],
                                start=(k == 0), stop=(k == 7)), [b_wgu_q[fq(f)], b_h2T], [PB[pu]], sig=(k == 7))
                        S.act(lambda f=f, pg=pg: nc.scalar.activation(out=sg[f % 2][:], in_=PS[pg][:, :], func=AF.Silu),
                              [PB[pg]], [b_sg[f % 2]])
                        S.dve(lambda f=f, pu=pu: nc.vector.tensor_tensor(out=actT[:, f, :], in0=sg[f % 2][:], in1=PS[pu][:, :],
                                                                         op=ALU.mult), [b_sg[f % 2], PB[pu]], [b_act[f]])
                    for j in range(4):
                        tt = c * 4 + j
                        i2 = tt % 2
                        S.dma("sp", lambda tt=tt, i2=i2: nc.sync.dma_start(out=xt[i2][:], in_=xs[tt * 128:(tt + 1) * 128, :]),
                              [b_xs[tt]], [b_xt[i2]])
                        for half in range(2):
                            pb = 6 + half
                            hs = slice(half * 512, (half + 1) * 512)
                            for f in range(NFC):
                                S.pe(lambda f=f, j=j, pb=pb, hs=hs: nc.tensor.matmul(
                                    PS[pb][:, :], lhsT=actT[:, f, j * 128:(j + 1) * 128], rhs=wd[:, f, hs],
                                    start=(f == 0), stop=(f == NFC - 1)), [b_act[f], b_wd], [PB[pb]], sig=(f == NFC - 1))
                            S.dve(lambda pb=pb, hs=hs: nc.vector.tensor_tensor(out=tmp[:, hs], in0=PS[pb][:, :],
                                                                               in1=MOD[G_F][:, hs], op=ALU.mult),
                                  [PB[pb], modb[G_F]], [b_tmp])
                        S.pool(lambda i2=i2: nc.gpsimd.tensor_tensor(out=xt[i2][:], in0=tmp[:], in1=xt[i2][:], op=ALU.add),
                               [b_tmp, b_xt[i2]], [b_xt[i2]])
                        if not final:
                            S.dma("sp", lambda tt=tt, i2=i2: nc.sync.dma_start(out=xs[tt * 128:(tt + 1) * 128, :], in_=xt[i2][:]),
                                  [b_xt[i2]], [b_xs[tt]])
                        else:
                            S.dve(lambda: nc.vector.memset(fss[:], 0.0), [], [b_fs])
                            S.act(lambda i2=i2: nc.scalar.activation(out=wk["junk"][:], in_=xt[i2][:], func=AF.Square,
                                                                     accum_out=fss[:, 0:1]), [b_xt[i2]], [wk["b_junk"], b_fs])
                            S.act(lambda: nc.scalar.activation(out=frs[:], in_=fss[:], func=AF.Sqrt, bias=eps6[:, 0:1],
                                                               scale=1.0 / D), [b_fs, cb], [b_fs])
                            S.dve(lambda: nc.vector.reciprocal(out=frs[:], in_=frs[:]), [b_fs], [b_fs])
                            S.dve(lambda i2=i2: nc.vector.scalar_tensor_tensor(out=tmp[:], in0=xt[i2][:], scalar=frs[:, 0:1],
                                                                               in1=fn[:], op0=ALU.mult, op1=ALU.mult),
                                  [b_xt[i2], b_fs, b_fn], [b_tmp])
                            S.dma("sp", lambda tt=tt: nc.sync.dma_start(out=y_out[tt * 128:(tt + 1) * 128, :], in_=tmp[:]),
                                  [b_tmp], [b_xs[tt]], is_out=True)
                S.barrier()

        def phase_ret():
            gam = [1.0 - 2.0 ** (-5.0 - h) for h in range(4)]
            with contextlib.ExitStack() as ph:
                psb = lambda name, shape, dt: ph.enter_context(sbt(name, shape, dt))
                hT = psb("r_hT", [128, 8, S_LEN], BF16)
                b_hT = [Buf() for _ in range(NQC)]
                with contextlib.ExitStack() as ph1:
                    wk = make_norm_wk(ph1, "r_")
                    xt = [ph1.enter_context(sbt("r_xt%d" % i, [128, D], F32)) for i in range(2)]
                    b_xt = [Buf(), Buf()]
                    ag = ada_gen(1, ph1)
                    for _ in range(4):
                        next(ag)
                    for tt in range(NT):
                        S.dma("sp", lambda tt=tt: nc.sync.dma_start(out=xt[tt % 2][:], in_=xs[tt * 128:(tt + 1) * 128, :]),
                              [b_xs[tt]], [b_xt[tt % 2]])
                        norm_mod_transpose(lambda t: xt[t % 2][:], lambda t: b_xt[t % 2], A_M, B_M, hT,
                                           lambda t: b_hT[t // 4], lambda t: t * 128, [tt], wk)
                        if tt % 4 == 3:
                            next(ag, None)
                    for _ in ag:
                        pass
                    S.barrier()
                cosT = psb("r_cos", [128, S_LEN], BF16)
                sinT = psb("r_sin", [128, S_LEN], BF16)
                b_tab = Buf()
                rope_tables(ph, "r_", "inv_ret", cosT, sinT, b_tab)
                wqk = psb("r_wqk", [128, 8, 512], BF16)
                wv = psb("r_wv", [128, 8, 512], BF16)
                b_wqk, b_wv = Buf(), Buf()
                qT = psb("r_qT", [128, 2, S_LEN], BF16)
                kT = psb("r_kT", [128, 2, S_LEN], BF16)
                vt = psb("r_v", [128, NT, 512], BF16)
                b_q, b_k, b_v = Buf(), Buf(), Buf()
                Gf = psb("r_Gf", [128, 512], F32)
                Gm = psb("r_Gm", [128, 512], F32)
                gn = psb("r_gn", [128, 512], F32)
                b_G = Buf()
                t1 = psb("r_t1", [128, 512], F32)
                t2 = psb("r_t2", [128, 512], F32)
                b_t1, b_t2 = Buf(), Buf()
                p_sb = [psb("r_p%d" % i, [128, 512], BF16) for i in range(2)]
                b_p = [Buf(), Buf()]
                o_sb, g_sb = t1, t2
                u_sb = psb("r_u", [128, 512], BF16)
                uTq = [psb("r_uT%d" % i, [128, 4, 128], BF16) for i in range(2)]
                b_uTq = [Buf(), Buf()]
                b_o, b_g, b_u = b_t1, b_t2, Buf()
                oq = [psb("r_oq%d" % i, [128, 512], BF16) for i in range(4)]
                b_oq = [Buf() for _ in range(4)]
                stq = [psb("r_st%d" % i, [128, 8], F32) for i in range(4)]
                b_stq = [Buf() for _ in range(4)]
                wsrc = I["ret_w_in"].rearrange("(k p) n -> p k n", p=128)
                for h in range(4):
                    S.dma("pool", lambda h=h: nc.gpsimd.dma_start(out=wqk[:, :, 0:256], in_=wsrc[:, :, h * 256:(h + 1) * 256]),
                          [], [b_wqk])
                    S.dma("pool", lambda h=h: nc.gpsimd.dma_start(out=wqk[:, :, 256:512],
                                                                  in_=wsrc[:, :, D + h * 256:D + (h + 1) * 256]), [], [b_wqk])
                    S.dma("pool", lambda h=h: nc.gpsimd.dma_start(out=wv[:], in_=wsrc[:, :, 2 * D + h * 512:2 * D + (h + 1) * 512]),
                          [], [b_wv])
                    S.dma("sp", lambda h=h: nc.sync.dma_start(out=Gf[:], in_=C["ret_gf"][h]), [], [b_G])
                    S.dma("sp", lambda h=h: nc.sync.dma_start(out=Gm[:], in_=C["ret_gm"][h]), [], [b_G])
                    S.dma("sp", lambda h=h: nc.sync.dma_start(
                        out=gn[:], in_=I["ret_gn"][0:1, h * 512:(h + 1) * 512].partition_broadcast(128)), [], [b_G])
                    for (c0, dst, bdst) in ((0, qT, b_q), (256, kT, b_k)):
                        for tc in range(NQC):
                            cols = slice(tc * 512, (tc + 1) * 512)
                            for dc in range(2):
                                for k in range(8):
                                    S.pe(lambda k=k, dc=dc, c0=c0, cols=cols: nc.tensor.matmul(
                                        PS[dc][:, :], lhsT=wqk[:, k, c0 + dc * 128:c0 + (dc + 1) * 128], rhs=hT[:, k, cols],
                                        start=(k == 0), stop=(k == 7)), [b_wqk, b_hT[tc]], [PB[dc]], sig=(k == 7))
                            S.dve(lambda cols=cols: nc.vector.tensor_tensor(out=t1[:], in0=PS[0][:, :], in1=cosT[:, cols],
                                                                            op=ALU.mult), [PB[0], b_tab], [b_t1])
                            S.dve(lambda cols=cols: nc.vector.tensor_tensor(out=t2[:], in0=PS[1][:, :], in1=sinT[:, cols],
                                                                            op=ALU.mult), [PB[1], b_tab], [b_t2])
                            S.pool(lambda cols=cols, dst=dst: nc.gpsimd.tensor_tensor(out=dst[:, 0, cols], in0=t1[:], in1=t2[:],
                                                                                      op=ALU.subtract), [b_t1, b_t2], [bdst])
                            S.dve(lambda cols=cols: nc.vector.tensor_tensor(out=t1[:], in0=PS[1][:, :], in1=cosT[:, cols],
                                                                            op=ALU.mult), [PB[1], b_tab], [b_t1])
                            S.dve(lambda cols=cols: nc.vector.tensor_tensor(out=t2[:], in0=PS[0][:, :], in1=sinT[:, cols],
                                                                            op=ALU.mult), [PB[0], b_tab], [b_t2])
                            S.pool(lambda cols=cols, dst=dst: nc.gpsimd.tensor_tensor(out=dst[:, 1, cols], in0=t1[:], in1=t2[:],
                                                                                      op=ALU.add), [b_t1, b_t2], [bdst])
                    for tt in range(NT):
                        pa = 2 + tt % 2
                        for k in range(8):
                            S.pe(lambda k=k, tt=tt, pa=pa: nc.tensor.matmul(
                                PS[pa][:, :], lhsT=hT[:, k, tt * 128:(tt + 1) * 128], rhs=wv[:, k, :], start=(k == 0),
                                stop=(k == 7)), [b_wv, b_hT[tt // 4]], [PB[pa]], sig=(k == 7))
                        S.act(lambda tt=tt, pa=pa: nc.scalar.copy(out=vt[:, tt, :], in_=PS[pa][:, :]), [PB[pa]], [b_v])
                    S.dma("pool", lambda h=h: nc.gpsimd.dma_start(out=wqk[:], in_=wsrc[:, :, 4 * D + h * 512:4 * D + (h + 1) * 512]),
                          [], [b_wqk])
                    def ret_main(qc):
                        nk = 4 * qc + 4

                        def r1(kt):
                            j = kt - 4 * qc
                            c_lo = 128 * j if j >= 0 else 0
                            n = 512 - c_lo
                            lc = slice(c_lo, 512)
                            qcols = slice(qc * 512 + c_lo, (qc + 1) * 512)
                            kcols = slice(kt * 128, (kt + 1) * 128)
                            pz = kt % 2
                            for dc in range(2):
                                S.pe(lambda dc=dc: nc.tensor.matmul(PS[pz][:, lc], lhsT=kT[:, dc, kcols], rhs=qT[:, dc, qcols],
                                                                    start=(dc == 0), stop=(dc == 1)), [b_k, b_q], [PB[pz]],
                                     sig=(dc == 1))
                            scal = 1.0 if j >= 0 else float(gam[h] ** (128.0 * (4 * qc - kt)))
                            G_ = Gm if j >= 0 else Gf
                            S.dve(lambda: nc.vector.scalar_tensor_tensor(out=p_sb[pz][:, lc], in0=PS[pz][:, lc], scalar=scal,
                                                                         in1=G_[:, 0:n], op0=ALU.mult, op1=ALU.mult),
                                  [PB[pz], b_G], [b_p[pz]])

                        def r2(kt):
                            j = kt - 4 * qc
                            pz = kt % 2
                            for qt in range(max(j, 0), 4):
                                S.pe(lambda qt=qt: nc.tensor.matmul(PS[2 + qt][:, :], lhsT=p_sb[pz][:, qt * 128:(qt + 1) * 128],
                                                                    rhs=vt[:, kt, :], start=(kt == 0), stop=(kt == 4 * qc + qt)),
                                     [b_p[pz], b_v], [PB[2 + qt]], sig=True)

                        for stp in range(nk + 1):
                            if 0 <= stp - 1 < nk:
                                r2(stp - 1)
                            if stp < nk:
                                r1(stp)
                            yield

                    def ret_epi(qc):
                        for qt in range(4):
                            po = 2 + qt
                            S.dve(lambda qt=qt: nc.vector.memset(stq[qt][:], 0.0), [], [b_stq[qt]])
                            S.act(lambda qt=qt, po=po: nc.scalar.activation(out=oq[qt][:], in_=PS[po][:, :], func=AF.Copy,
                                                                            accum_out=stq[qt][:, 0:1]), [PB[po]],
                                  [b_oq[qt], b_stq[qt]])
                        yield
                        for qt in range(4):
                            tt = 4 * qc + qt
                            st = stq[qt]
                            b_st = b_stq[qt]
                            S.act(lambda qt=qt, st=st: nc.scalar.activation(out=g_sb[:], in_=oq[qt][:], func=AF.Square,
                                                                            accum_out=st[:, 1:2]), [b_oq[qt]], [b_g, b_st])
                            S.dve(lambda st=st: nc.vector.tensor_scalar(out=st[:, 2:3], in0=st[:, 0:1], scalar1=1.0 / 512,
                                                                        scalar2=None, op0=ALU.mult), [b_st], [b_st])
                            S.dve(lambda st=st: nc.vector.tensor_tensor(out=st[:, 3:4], in0=st[:, 2:3], in1=st[:, 2:3],
                                                                        op=ALU.mult), [b_st], [b_st])
                            S.dve(lambda st=st: nc.vector.scalar_tensor_tensor(out=st[:, 4:5], in0=st[:, 1:2], scalar=1.0 / 512,
                                                                               in1=st[:, 3:4], op0=ALU.mult, op1=ALU.subtract),
                                  [b_st], [b_st])
                            yield
                            S.act(lambda st=st: nc.scalar.activation(out=st[:, 5:6], in_=st[:, 4:5], func=AF.Sqrt,
                                                                     bias=eps5[:, 0:1], scale=1.0), [b_st, cb], [b_st])
                            S.dve(lambda st=st: nc.vector.reciprocal(out=st[:, 6:7], in_=st[:, 5:6]), [b_st], [b_st])
                            S.dve(lambda qt=qt, st=st: nc.vector.tensor_scalar(out=o_sb[:], in0=oq[qt][:], scalar1=st[:, 2:3],
                                                                               scalar2=st[:, 6:7], op0=ALU.subtract, op1=ALU.mult),
                                  [b_oq[qt], b_st], [b_o])
                            S.pool(lambda: nc.gpsimd.tensor_tensor(out=o_sb[:], in0=o_sb[:], in1=gn[:], op=ALU.mult),
                                   [b_o, b_G], [b_o])
                            yield
                            for k in range(8):
                                S.pe(lambda k=k, tt=tt: nc.tensor.matmul(PS[6][:, :], lhsT=hT[:, k, tt * 128:(tt + 1) * 128],
                                                                        rhs=wqk[:, k, :], start=(k == 0), stop=(k == 7)),
                                     [b_wqk, b_hT[tt // 4]], [PB[6]], sig=(k == 7))
                            S.act(lambda: nc.scalar.activation(out=g_sb[:], in_=PS[6][:, :], func=AF.Silu), [PB[6]], [b_g])
                            S.dve(lambda: nc.vector.tensor_tensor(out=u_sb[:], in0=o_sb[:], in1=g_sb[:], op=ALU.mult),
                                  [b_o, b_g], [b_u])
                            yield
                            pst = PS[7].bitcast(BF16)
                            for jj in range(4):
                                S.pe(lambda jj=jj, pst=pst: nc.tensor.transpose(out=pst[:, jj * 128:(jj + 1) * 128],
                                                                               in_=u_sb[:, jj * 128:(jj + 1) * 128],
                                                                               identity=ident_bf[:]), [b_u, cb], [PB[7]], sig=(jj == 3))
                            ub = qt % 2
                            S.act(lambda ub=ub, pst=pst: nc.scalar.activation(
                                out=uTq[ub][:, :, :], in_=pst[:, 0:512].rearrange("p (j t) -> p j t", j=4), func=AF.Copy),
                                [PB[7]], [b_uTq[ub]])
                            S.dma("sp", lambda ub=ub, tt=tt: nc.sync.dma_start(
                                out=uT_d[h * 512:(h + 1) * 512, tt * 128:(tt + 1) * 128].rearrange("(j p) t -> p j t", p=128),
                                in_=uTq[ub][:]), [b_uTq[ub]], [])
                            yield

                    gens = [ret_main(0)]
                    for qc in range(NQC):
                        while gens:
                            for g_ in list(gens):
                                try:
                                    next(g_)
                                except StopIteration:
                                    gens.remove(g_)
                        gens = [ret_epi(qc)]
                        if qc + 1 < NQC:
                            gens.append(ret_main(qc + 1))
                    while gens:
                        for g_ in list(gens):
                            try:
                                next(g_)
                            except StopIteration:
                                gens.remove(g_)
                S.barrier()

        phase_att()
        if stop_after in (None, "l0"):
            phase_outproj(I["x"], oT_d, 8, I["att_w_o"])
            phase_ffn(0, final=(stop_after == "l0"))
        if stop_after is None:
            phase_ret()
            phase_outproj(xs, uT_d, 16, I["ret_w_o"])
            phase_ffn(1, final=True)
        S.finish()
    return nc, S


def _prep_inputs(inputs, b):
    m = {}
    f = lambda a: np.ascontiguousarray(np.asarray(a, dtype=np.float32))
    m["x"] = f(inputs["x"][b])
    m["c"] = f(inputs["c"][b]).reshape(128, 8)
    m["ada_w"] = f(inputs["ada_w"])
    m["ada_b"] = f(inputs["ada_b"])
    m["norm_gains"] = f(inputs["norm_gains"])
    m["att_w_qkv"] = f(inputs["att_w_qkv"][0])
    m["att_w_o"] = f(inputs["att_w_o"][0])
    m["ret_w_in"] = f(inputs["ret_w_in"][0])
    m["ret_gn"] = f(inputs["ret_gn"]).reshape(1, 2 * D)
    m["ret_w_o"] = f(inputs["ret_w_o"][0])
    m["ffn_w_gate_up"] = f(inputs["ffn_w_gate_up"])
    m["ffn_w_down"] = f(inputs["ffn_w_down"])
    m["final_norm"] = f(inputs["final_norm"]).reshape(1, D)
    for k, v in _consts().items():
        if k == "ret_cd":
            continue
        m["k_" + k] = np.ascontiguousarray(v)
    return m


def kernel(**inputs):
    nc, _ = build_program()
    maps = [_prep_inputs(inputs, b) for b in range(4)]
    res = run_bass_kernel_spmd(nc, maps, core_ids=list(range(4)))
    out = np.stack([np.asarray(res.results[b]["y"], dtype=np.float32) for b in range(4)], axis=0)
    return out
```

```python
import contextlib
import math
import numpy as np
import ml_dtypes
import concourse.bass as bass
import concourse.mybir as mybir
from concourse.bass_utils import run_bass_kernel_spmd

F32 = mybir.dt.float32
BF16 = mybir.dt.bfloat16
AF = mybir.ActivationFunctionType
ALU = mybir.AluOpType
AX = mybir.AxisListType

S_LEN = 4096
D = 1024
NT = S_LEN // 128
NQC = S_LEN // 512
DFF = 2816
NFC = DFF // 128
BIG = 30000.0
PI = math.pi


class Buf:
    __slots__ = ("name", "w", "r")

    def __init__(self, name=""):
        self.name = name
        self.w = None
        self.r = {}


class Op:
    __slots__ = ("eng", "is_dma", "sem", "val", "idx")


class Sched:
    def __init__(self, nc, es, n_dma_sems=32, epoch=30000):
        self.nc = nc
        self.es = es
        self.engs = {"pe": nc.tensor, "act": nc.scalar, "dve": nc.vector, "pool": nc.gpsimd, "sp": nc.sync}
        self.n = 0
        self.epoch = epoch
        self.cnt = {e: 0 for e in ("pe", "act", "dve", "pool")}
        self.cur_sem = {}
        for e in self.cnt:
            self.cur_sem[e] = es.enter_context(nc.semaphore("s_" + e + "0"))
        self.nsem = {e: 1 for e in self.cnt}
        self.dma_sems = [es.enter_context(nc.semaphore("s_dma%d" % i)) for i in range(n_dma_sems)]
        self.dma_last = [None] * n_dma_sems
        self.dma_cnt = [0] * n_dma_sems
        self.dma_rr = 0
        self.n_hw = n_dma_sems
        self.waited = {}
        self.pending_pe = []
        self.last_op = {}
        self.out_dmas = []
        self.n_wait = 0

    def _emit_wait(self, eng, d):
        assert d.sem is not None, "dependency on un-signalled PE op"
        key = (eng, id(d.sem))
        if self.waited.get(key, -1) >= d.val:
            return
        self.waited[key] = d.val
        self.engs[eng].wait_ge(d.sem, d.val)
        self.n_wait += 1

    def _deps(self, eng, reads, writes, is_dma):
        deps = {}
        for b in reads:
            if b.w is not None:
                deps[b.w.idx] = b.w
        for b in writes:
            if b.w is not None:
                deps[b.w.idx] = b.w
            for r in b.r.values():
                deps[r.idx] = r
        for d in deps.values():
            if (not d.is_dma) and d.eng == "pe" and eng == "pe" and not is_dma:
                continue
            self._emit_wait(eng, d)

    def _record(self, op, eng, reads, writes):
        rkey = ("dma", op.idx) if op.is_dma else eng
        for b in reads:
            b.r[rkey] = op
        for b in writes:
            b.w = op
            b.r = {}
        self.last_op[rkey if not op.is_dma else ("dmaq", eng)] = op

    def op(self, eng, fn, reads=(), writes=(), sig=True):
        self._deps(eng, reads, writes, False)
        inst = fn()
        op = Op()
        op.eng = eng
        op.is_dma = False
        op.idx = self.n
        self.n += 1
        op.sem = None
        op.val = None
        if eng == "pe" and not sig:
            self.pending_pe.append(op)
        else:
            if self.cnt[eng] >= self.epoch:
                self.cur_sem[eng] = self.es.enter_context(self.nc.semaphore("s_%s%d" % (eng, self.nsem[eng])))
                self.nsem[eng] += 1
                self.cnt[eng] = 0
            self.cnt[eng] += 1
            op.sem = self.cur_sem[eng]
            op.val = self.cnt[eng]
            inst.then_inc(op.sem, 1)
            if eng == "pe":
                for p in self.pending_pe:
                    p.sem = op.sem
                    p.val = op.val
                self.pending_pe = []
        self._record(op, eng, reads, writes)
        return op

    def pe(self, fn, r=(), w=(), sig=False):
        return self.op("pe", fn, r, w, sig)

    def act(self, fn, r=(), w=()):
        return self.op("act", fn, r, w)

    def dve(self, fn, r=(), w=()):
        return self.op("dve", fn, r, w)

    def pool(self, fn, r=(), w=()):
        return self.op("pool", fn, r, w)

    def dma(self, q, fn, reads=(), writes=(), is_out=False):
        if q == "pool":
            self.dma_sems.append(self.es.enter_context(self.nc.semaphore("s_sw%d" % len(self.dma_sems))))
            self.dma_last.append(None)
            self.dma_cnt.append(0)
            slot = len(self.dma_sems) - 1
        else:
            slot = self.dma_rr
            self.dma_rr = (self.dma_rr + 1) % self.n_hw
        prev = self.dma_last[slot]
        if prev is not None:
            self._emit_wait(q, prev)
        self._deps(q, reads, writes, True)
        inst = fn()
        op = Op()
        op.eng = q
        op.is_dma = True
        op.idx = self.n
        self.n += 1
        self.dma_cnt[slot] += 16
        op.sem = self.dma_sems[slot]
        op.val = self.dma_cnt[slot]
        inst.then_inc(op.sem, 16)
        self.dma_last[slot] = op
        self._record(op, q, reads, writes)
        if is_out:
            self.out_dmas.append(op)
        return op

    def barrier(self):
        assert not self.pending_pe
        lasts = [o for k, o in self.last_op.items() if not (isinstance(k, tuple))]
        dmas = [o for o in self.dma_last if o is not None]
        for eng in ("pe", "act", "dve", "pool", "sp"):
            for d in lasts + dmas:
                if d.is_dma or d.eng != eng:
                    self._emit_wait(eng, d)

    def finish(self):
        for d in self.dma_last:
            if d is not None:
                self._emit_wait("sp", d)


def _consts():
    bf = ml_dtypes.bfloat16
    j = np.arange(128)[:, None]
    s = np.arange(128)[None, :]
    c = {}
    c["ident_bf"] = np.eye(128, dtype=np.float32).astype(bf)
    c["ident_f"] = np.eye(128, dtype=np.float32)
    c["tri_neg"] = np.where(j >= s, -1.0, 0.0).astype(bf)
    c["ones_neg"] = np.full((128, 128), -1.0, np.float32).astype(bf)
    c["m01_strict"] = np.where(j < s, 1.0, 0.0).astype(bf)
    c["nm_strict"] = np.where(j < s, 0.0, -BIG).astype(bf)
    c["nm_causal"] = np.where(j <= s, 0.0, -BIG).astype(bf)
    c["pos"] = np.arange(S_LEN, dtype=np.float32)[None, :]
    inv64 = (10000.0 ** (-np.arange(0, 64, 2, dtype=np.float32) / 64.0)).astype(np.float32)
    c["inv_att"] = np.tile(inv64, 4)[:, None].astype(np.float32)
    inv256 = (10000.0 ** (-np.arange(0, 256, 2, dtype=np.float32) / 256.0)).astype(np.float32)
    c["inv_ret"] = inv256[:, None].astype(np.float32)
    kaug = np.zeros((32, S_LEN), np.float32)
    for n in range(16):
        kaug[1 + n, n * 256:(n + 1) * 256] = 1.0
    c["kaug"] = kaug.astype(bf)
    hh = np.arange(4, dtype=np.float64)
    g = 1.0 - 2.0 ** (-5.0 - hh)
    n = np.arange(128, dtype=np.float64)
    dm = np.zeros((4, 128, 128), np.float32)
    zk = np.zeros((128, 4), np.float32)
    xi = np.zeros((128, 4), np.float32)
    for h in range(4):
        z = g[h] ** (-(n + 1.0)) / 16.0
        zk[:, h] = z
        xi[:, h] = g[h] ** (n + 1.0)
        dm[h] = np.where(j <= s, z[:, None], 0.0)
    c["ret_dm"] = dm
    cc = np.arange(512, dtype=np.float64)[None, :]
    ss_ = np.arange(128, dtype=np.float64)[:, None]
    gf = np.zeros((4, 128, 512), np.float32)
    gm = np.zeros((4, 128, 512), np.float32)
    for h in range(4):
        full = g[h] ** (cc - ss_) / 16.0
        gf[h] = full
        gm[h] = np.where(cc >= ss_, full, 0.0)
    c["ret_gf"] = gf
    c["ret_gm"] = gm
    c["ret_zk"] = zk
    c["ret_xi"] = xi
    c["ret_cd"] = (g ** 128.0).astype(np.float32)
    return c


_CONST_SPECS = [
    ("ident_bf", [128, 128], BF16), ("ident_f", [128, 128], F32), ("tri_neg", [128, 128], BF16),
    ("ones_neg", [128, 128], BF16), ("m01_strict", [128, 128], BF16), ("nm_strict", [128, 128], BF16),
    ("nm_causal", [128, 128], BF16), ("pos", [1, S_LEN], F32), ("inv_att", [128, 1], F32),
    ("inv_ret", [128, 1], F32), ("kaug", [32, S_LEN], BF16), ("ret_dm", [4, 128, 128], F32),
    ("ret_zk", [128, 4], F32), ("ret_xi", [128, 4], F32), ("ret_gf", [4, 128, 512], F32), ("ret_gm", [4, 128, 512], F32),
]

_IN_SPECS = [
    ("x", [S_LEN, D]), ("c", [128, 8]), ("ada_w", [2, D, 6 * D]), ("ada_b", [2, 6 * D]),
    ("norm_gains", [2, 2, D]), ("att_w_qkv", [D, 3 * D]), ("att_w_o", [D, D]), ("ret_w_in", [D, 6 * D]),
    ("ret_gn", [1, 2 * D]), ("ret_w_o", [2 * D, D]), ("ffn_w_gate_up", [2, D, 2 * DFF]),
    ("ffn_w_down", [2, DFF, D]), ("final_norm", [1, D]),
]


def build_program(stop_after=None, debug=False):
    nc = bass.Bass("TRN2", target_bir_lowering=False)
    I = {}
    for name, shape in _IN_SPECS:
        I[name] = nc.dram_tensor(name, shape, F32, kind="ExternalInput").ap()
    C = {}
    for name, shape, dt in _CONST_SPECS:
        C[name] = nc.dram_tensor("k_" + name, shape, dt, kind="ExternalInput").ap()
    y_out = nc.dram_tensor("y", [S_LEN, D], F32, kind="ExternalOutput").ap()
    xs = nc.dram_tensor("xs", [S_LEN, D], F32).ap()
    oT_d = nc.dram_tensor("oT_d", [D, S_LEN], BF16).ap()
    uT_d = nc.dram_tensor("uT_d", [2 * D, S_LEN], BF16).ap()
    dbg = {}
    if debug:
        dbg["mod"] = nc.dram_tensor("dbg_mod", [128, 6 * D], F32, kind="ExternalOutput").ap()
        if stop_after != "ada":
            dbg["hT"] = nc.dram_tensor("dbg_hT", [128, 8 * S_LEN], BF16, kind="ExternalOutput").ap()
        if stop_after in ("sb0", "moba0", "att", "l0"):
            dbg["oT"] = nc.dram_tensor("dbg_oT", [D, S_LEN], BF16, kind="ExternalOutput").ap()

    _uid = [0]

    def sbt(name, shape, dt):
        _uid[0] += 1
        return nc.sbuf_tensor("%s_u%d" % (name, _uid[0]), shape, dt)

    with contextlib.ExitStack() as es:
        S = Sched(nc, es)
        sb = lambda name, shape, dt: es.enter_context(sbt(name, shape, dt))
        PS = [es.enter_context(nc.psum_tensor("ps%d" % i, [128, 512], F32)) for i in range(8)]
        PB = [Buf("ps%d" % i) for i in range(8)]

        ident_bf = sb("ident_bf", [128, 128], BF16)
        ident_f = sb("ident_f", [128, 128], F32)
        tri_neg = sb("tri_neg", [128, 128], BF16)
        ones_neg = sb("ones_neg", [128, 128], BF16)
        m01 = sb("m01", [128, 128], BF16)
        nm_strict = sb("nm_strict", [128, 128], BF16)
        nm_causal = sb("nm_causal", [128, 128], BF16)
        ones_f = sb("ones_f", [128, 128], F32)
        negpi = sb("negpi", [128, 1], F32)
        cb = Buf("consts")
        for t, nme in ((ident_bf, "ident_bf"), (ident_f, "ident_f"), (tri_neg, "tri_neg"), (ones_neg, "ones_neg"),
                       (m01, "m01_strict"), (nm_strict, "nm_strict"), (nm_causal, "nm_causal")):
            S.dma("sp", lambda t=t, nme=nme: nc.sync.dma_start(out=t[:], in_=C[nme][:, :]), [], [cb])
        S.dve(lambda: nc.vector.memset(ones_f[:], 1.0), [], [cb])
        S.dve(lambda: nc.vector.memset(negpi[:], -PI), [], [cb])
        zeros_bf = sb("zeros_bf", [128, 512], BF16)
        S.dve(lambda: nc.vector.memset(zeros_bf[:], 0.0), [], [cb])
        eps6 = sb("eps6", [128, 1], F32)
        eps5 = sb("eps5", [128, 1], F32)
        S.dve(lambda: nc.vector.memset(eps6[:], 1e-6), [], [cb])
        S.dve(lambda: nc.vector.memset(eps5[:], 1e-5), [], [cb])

        MOD = [sb("mod%d" % i, [128, D], F32) for i in range(6)]
        modb = [Buf("mod%d" % i) for i in range(6)]

        def ada_gen(layer, ph):
            psb = lambda name, shape, dt: ph.enter_context(sbt(name, shape, dt))
            ccol = psb("ccol", [128, 8], F32)
            scol = psb("scol", [128, 8], F32)
            rep = psb("rep", [128, 8, 128], F32)
            wch = [psb("adaw%d" % i, [128, 8, 512], F32) for i in range(2)]
            bch = [psb("adab%d" % i, [128, 512], F32) for i in range(2)]
            gt = [psb("adag%d" % i, [128, D], F32) for i in range(2)]
            b_c, b_rep = Buf(), Buf()
            b_w = [Buf(), Buf()]
            b_b = [Buf(), Buf()]
            b_g = [Buf(), Buf()]
            S.dma("sp", lambda: nc.sync.dma_start(out=ccol[:], in_=I["c"][:, :]), [], [b_c])
            S.act(lambda: nc.scalar.activation(out=scol[:], in_=ccol[:], func=AF.Silu), [b_c], [b_c])
            for k in range(8):
                S.dve(lambda k=k: nc.vector.tensor_scalar(out=rep[:, k, :], in0=ones_f[:], scalar1=scol[:, k:k + 1],
                                                          scalar2=None, op0=ALU.mult), [b_c, cb], [b_rep])
            for gi in range(2):
                S.dma("sp", lambda gi=gi: nc.sync.dma_start(
                    out=gt[gi][:], in_=I["norm_gains"][layer, gi:gi + 1, :].partition_broadcast(128)), [], [b_g[gi]])
            wv = I["ada_w"][layer].rearrange("(p k) n -> p k n", k=8)
            for ci in range(12):
                w_, bb_ = wch[ci % 2], bch[ci % 2]
                S.dma("sp", lambda ci=ci, w_=w_: nc.sync.dma_start(out=w_[:], in_=wv[:, :, ci * 512:(ci + 1) * 512]),
                      [], [b_w[ci % 2]])
                S.dma("sp", lambda ci=ci, bb_=bb_: nc.sync.dma_start(
                    out=bb_[:], in_=I["ada_b"][layer:layer + 1, ci * 512:(ci + 1) * 512].partition_broadcast(128)),
                    [], [b_b[ci % 2]])
                pb = ci % 2
                for k in range(8):
                    S.pe(lambda k=k, w_=w_, pb=pb: nc.tensor.matmul(PS[pb][:, :], lhsT=rep[:, k, :], rhs=w_[:, k, :],
                                                                    start=(k == 0), stop=(k == 7)),
                         [b_rep, b_w[ci % 2]], [PB[pb]], sig=(k == 7))
                jm, half = ci // 2, ci % 2
                S.dve(lambda jm=jm, half=half, pb=pb, bb_=bb_: nc.vector.tensor_tensor(
                    out=MOD[jm][:, half * 512:(half + 1) * 512], in0=PS[pb][:, :], in1=bb_[:], op=ALU.add),
                    [PB[pb], b_b[ci % 2]], [modb[jm]])
                for (js, gi, cdone) in ((1, 0, 3), (4, 1, 9)):
                    if ci == cdone:
                        S.dve(lambda js=js, gi=gi: nc.vector.scalar_tensor_tensor(
                            out=MOD[js][:], in0=MOD[js][:], scalar=1.0, in1=gt[gi][:], op0=ALU.add, op1=ALU.mult),
                            [modb[js], b_g[gi]], [modb[js]])
                yield

        A_M, B_M, G_M, A_F, B_F, G_F = 1, 0, 2, 4, 3, 5

        def norm_mod_transpose(x_src_ap, xbuf_token, a_idx, b_idx, hT, hT_buf, tt_glob_cols, tiles, wk):
            for tt in tiles:
                xt = x_src_ap(tt)
                xb = xbuf_token(tt)
                i2 = tt % 2
                ss, rstd, junk, tmp, hb = wk["ss"][i2], wk["rstd"][i2], wk["junk"], wk["tmp"][i2], wk["hb"][i2]
                b_ss, b_junk, b_tmp, b_hb = wk["b_ss"][i2], wk["b_junk"], wk["b_tmp"][i2], wk["b_hb"][i2]
                S.dve(lambda ss=ss: nc.vector.memset(ss[:], 0.0), [], [b_ss])
                S.act(lambda xt=xt, ss=ss, junk=junk: nc.scalar.activation(out=junk[:], in_=xt, func=AF.Square,
                                                                            accum_out=ss[:, 0:1]), [xb], [b_junk, b_ss])
                S.act(lambda ss=ss, rstd=rstd: nc.scalar.activation(out=rstd[:], in_=ss[:], func=AF.Sqrt, bias=eps6[:, 0:1],
                                                                    scale=1.0 / D), [b_ss, cb], [b_ss])
                S.dve(lambda rstd=rstd: nc.vector.reciprocal(out=rstd[:], in_=rstd[:]), [b_ss], [b_ss])
                S.dve(lambda xt=xt, rstd=rstd, tmp=tmp: nc.vector.scalar_tensor_tensor(
                    out=tmp[:], in0=xt, scalar=rstd[:, 0:1], in1=MOD[a_idx][:], op0=ALU.mult, op1=ALU.mult),
                    [xb, b_ss, modb[a_idx]], [b_tmp])
                S.pool(lambda tmp=tmp, hb=hb: nc.gpsimd.tensor_tensor(out=hb[:], in0=tmp[:], in1=MOD[b_idx][:], op=ALU.add),
                       [b_tmp, modb[b_idx]], [b_hb])
                pb = wk["psT"][i2]
                pst = PS[pb].bitcast(BF16)
                for k in range(8):
                    S.pe(lambda k=k, hb=hb, pst=pst: nc.tensor.transpose(out=pst[:, k * 128:(k + 1) * 128],
                                                                        in_=hb[:, k * 128:(k + 1) * 128], identity=ident_bf[:]),
                         [b_hb, cb], [PB[pb]], sig=(k == 7))
                c0 = tt_glob_cols(tt)
                S.act(lambda pst=pst, c0=c0: nc.scalar.activation(
                    out=hT[:, :, c0:c0 + 128], in_=pst.rearrange("p (k t) -> p k t", k=8), func=AF.Copy),
                    [PB[pb]], [hT_buf(tt)])

        def make_norm_wk(ph, pfx, psT=(6, 7), single=False):
            psb = lambda name, shape, dt: ph.enter_context(sbt(pfx + name, shape, dt))
            if single:
                tmp_ = psb("tmp", [128, D], F32)
                hb_ = psb("hb", [128, D], BF16)
                bt, bh = Buf(), Buf()
                return {
                    "ss": [psb("ss%d" % i, [128, 1], F32) for i in range(2)],
                    "rstd": [psb("rstd%d" % i, [128, 1], F32) for i in range(2)],
                    "junk": psb("junk", [128, D], BF16), "tmp": [tmp_, tmp_], "hb": [hb_, hb_],
                    "b_ss": [Buf(), Buf()], "b_junk": Buf(), "b_tmp": [bt, bt], "b_hb": [bh, bh], "psT": psT,
                }
            return {
                "ss": [psb("ss%d" % i, [128, 1], F32) for i in range(2)],
                "rstd": [psb("rstd%d" % i, [128, 1], F32) for i in range(2)],
                "junk": psb("junk", [128, D], BF16),
                "tmp": [psb("tmp%d" % i, [128, D], F32) for i in range(2)],
                "hb": [psb("hb%d" % i, [128, D], BF16) for i in range(2)],
                "b_ss": [Buf(), Buf()], "b_junk": Buf(), "b_tmp": [Buf(), Buf()], "b_hb": [Buf(), Buf()],
                "psT": psT,
            }

        def rope_tables(ph, pfx, inv_name, cosT, sinT, b_tab):
            with contextlib.ExitStack() as tp:
                psb = lambda name, shape, dt: tp.enter_context(sbt(pfx + name, shape, dt))
                inv = psb("inv", [128, 1], F32)
                ang = psb("ang", [128, 1024], F32)
                yy = psb("yy", [128, 1024], F32)
                ni = psb("ni", [128, 1024], mybir.dt.int32)
                b_inv, b_ang, b_y, b_n = Buf(), Buf(), Buf(), Buf()
                S.dma("sp", lambda: nc.sync.dma_start(out=inv[:], in_=C[inv_name][:, :]), [], [b_inv])
                for ch in range(4):
                    cs = slice(ch * 1024, (ch + 1) * 1024)
                    S.dma("sp", lambda cs=cs: nc.sync.dma_start(out=ang[:], in_=C["pos"][0:1, cs].partition_broadcast(128)),
                          [], [b_ang])
                    S.dve(lambda: nc.vector.tensor_scalar(out=ang[:], in0=ang[:], scalar1=inv[:, 0:1], scalar2=None,
                                                          op0=ALU.mult), [b_inv, b_ang], [b_ang])
                    for (dst, off) in ((sinT, 0.0), (cosT, 0.5 * PI)):
                        S.dve(lambda off=off: nc.vector.tensor_scalar(out=yy[:], in0=ang[:], scalar1=1.0 / (2 * PI),
                                                                      scalar2=off / (2 * PI) + 0.5, op0=ALU.mult, op1=ALU.add),
                              [b_ang], [b_y])
                        S.dve(lambda: nc.vector.tensor_copy(out=ni[:], in_=yy[:]), [b_y], [b_n])
                        S.dve(lambda: nc.vector.tensor_copy(out=yy[:], in_=ni[:]), [b_n], [b_y])
                        S.dve(lambda: nc.vector.scalar_tensor_tensor(out=yy[:], in0=yy[:], scalar=-2 * PI, in1=ang[:],
                                                                     op0=ALU.mult, op1=ALU.add), [b_y, b_ang], [b_y])
                        if off != 0.0:
                            S.dve(lambda off=off: nc.vector.tensor_scalar(out=yy[:], in0=yy[:], scalar1=off, scalar2=None,
                                                                          op0=ALU.add), [b_y], [b_y])
                        S.dve(lambda cs=cs, dst=dst: nc.vector.tensor_scalar(out=dst[:, cs], in0=yy[:], scalar1=-PI,
                                                                             scalar2=2 * PI, op0=ALU.is_lt, op1=ALU.mult),
                              [b_y], [b_tab])
                        S.dve(lambda cs=cs, dst=dst: nc.vector.tensor_tensor(out=yy[:], in0=yy[:], in1=dst[:, cs], op=ALU.add),
                              [b_y, b_tab], [b_y])
                        S.dve(lambda cs=cs, dst=dst: nc.vector.tensor_scalar(out=dst[:, cs], in0=yy[:], scalar1=PI,
                                                                             scalar2=-2 * PI, op0=ALU.is_gt, op1=ALU.mult),
                              [b_y], [b_tab])
                        S.dve(lambda cs=cs, dst=dst: nc.vector.tensor_tensor(out=yy[:], in0=yy[:], in1=dst[:, cs], op=ALU.add),
                              [b_y, b_tab], [b_y])
                        S.act(lambda cs=cs, dst=dst: nc.scalar.activation(out=dst[:, cs], in_=yy[:], func=AF.Sin),
                              [b_y], [b_tab])
                S.barrier()

        def phase_att():
            with contextlib.ExitStack() as ph:
                psb = lambda name, shape, dt: ph.enter_context(sbt(name, shape, dt))
                hT = psb("hT", [128, 8, S_LEN], BF16)
                b_hT = [Buf("hT%d" % i) for i in range(NQC)]
                with contextlib.ExitStack() as ph1:
                    wk = make_norm_wk(ph1, "a_")
                    xt = [ph1.enter_context(sbt("a_xt%d" % i, [128, D], F32)) for i in range(2)]
                    b_xt = [Buf(), Buf()]
                    ag = ada_gen(0, ph1)
                    for _ in range(4):
                        next(ag)
                    for tt in range(NT):
                        S.dma("sp", lambda tt=tt: nc.sync.dma_start(out=xt[tt % 2][:], in_=I["x"][tt * 128:(tt + 1) * 128, :]),
                              [], [b_xt[tt % 2]])
                        norm_mod_transpose(lambda t: xt[t % 2][:], lambda t: b_xt[t % 2], A_M, B_M, hT,
                                           lambda t: b_hT[t // 4], lambda t: t * 128, [tt], wk)
                        if tt % 4 == 3:
                            next(ag, None)
                    for _ in ag:
                        pass
                    S.barrier()
                if debug:
                    S.dma("sp", lambda: nc.sync.dma_start(out=dbg["hT"][:, :], in_=hT[:].rearrange("p k t -> p (k t)")),
                          b_hT, [], is_out=True)
                if stop_after == "hT":
                    return
                cosT = psb("cosT", [128, S_LEN], F32)
                sinT = psb("sinT", [128, S_LEN], F32)
                b_tab = Buf("tab")
                rope_tables(ph, "a_", "inv_att", cosT, sinT, b_tab)

                wq = psb("wq", [128, 8, 128], BF16)
                wkk = psb("wk", [128, 8, 128], BF16)
                wv = psb("wv", [128, 8, 128], BF16)
                wqs = psb("wqs", [128, 8, 128], BF16)
                wks = psb("wks", [128, 8, 128], BF16)
                b_w = Buf("w")
                b_ws = Buf("ws")
                qT = [psb("qT%d" % i, [96, S_LEN], BF16) for i in range(2)]
                kT = [psb("kT%d" % i, [96, S_LEN], BF16) for i in range(2)]
                vv = psb("vv", [128, NT, 2, 65], BF16)
                b_q = [Buf(), Buf()]
                b_k = [Buf(), Buf()]
                b_v = Buf()
                t1 = psb("t1", [128, 512], F32)
                t2 = psb("t2", [128, 512], F32)
                t3 = t1
                b_t1, b_t2 = Buf(), Buf()
                b_t3 = b_t1
                e_sb = [[psb("e_sb%d_%d" % (h_, i), [128, 512], BF16) for i in range(2)] for h_ in range(2)]
                sp_sb = [[psb("sp_sb%d_%d" % (h_, i), [128, 512], BF16) for i in range(2)] for h_ in range(2)]
                a_sb = [[psb("a_sb%d_%d" % (h_, i), [128, 512], BF16) for i in range(2)] for h_ in range(2)]
                ssum = [[psb("ssum%d_%d" % (h_, i), [128, 512], BF16) for i in range(2)] for h_ in range(2)]
                o_sb = [[psb("o_sb%d_%d" % (h_, i), [64, 512], BF16) for i in range(2)] for h_ in range(2)]
                b_e = [[Buf(), Buf()] for _ in range(2)]
                b_sp = [[Buf(), Buf()] for _ in range(2)]
                b_a = [[Buf(), Buf()] for _ in range(2)]
                b_ssum = [[Buf(), Buf()] for _ in range(2)]
                b_o = [[Buf(), Buf()] for _ in range(2)]
                b_oT = [[Buf() for _ in range(NQC)] for _ in range(16)]
                kmean = psb("kmean", [64, 16], F32)
                kmean_hi = psb("kmean_hi", [64, 16], BF16)
                kmean_lo = psb("kmean_lo", [64, 16], BF16)
                kmr = psb("kmr", [64, 16], F32)
                b_km = Buf()
                gpad = [psb("gpad%d" % i, [128, 16], F32) for i in range(2)]
                top8 = [psb("top8%d" % i, [128, 8], F32) for i in range(2)]
                xaug = [psb("xaug%d" % i, [128, 32], F32) for i in range(2)]
                b_g = [Buf(), Buf()]
                rden = [psb("rden%d" % i, [1, 512], F32) for i in range(2)]
                of_sb = [psb("of_sb%d" % i, [64, 512], F32) for i in range(2)]
                b_rden, b_of = [Buf(), Buf()], [Buf(), Buf()]
                ones_row = psb("ones_row", [65, 64], F32)
                S.dve(lambda: nc.vector.memset(ones_row[:], 1.0), [], [cb])
                S.dve(lambda: nc.vector.memset(vv[:], 1.0), [], [b_v])
                for i in range(2):
                    S.dma("sp", lambda i=i: nc.sync.dma_start(out=kT[i][64:96, :], in_=C["kaug"][:, :]), [], [b_k[i]])

                wsrc = I["att_w_qkv"].rearrange("(k p) n -> p k n", p=128)

                def project_pair(hp, moba):
                    c0 = hp * 128
                    S.dma("pool", lambda: nc.gpsimd.dma_start(out=wq[:], in_=wsrc[:, :, c0:c0 + 128]), [], [b_w])
                    S.dma("pool", lambda: nc.gpsimd.dma_start(out=wkk[:], in_=wsrc[:, :, D + c0:D + c0 + 128]), [], [b_w])
                    S.dma("pool", lambda: nc.gpsimd.dma_start(out=wv[:], in_=wsrc[:, :, 2 * D + c0:2 * D + c0 + 128]), [], [b_w])
                    if moba:
                        for (src, dst) in ((wq, wqs), (wkk, wks)):
                            for hh in range(2):
                                b0 = hh * 64
                                S.act(lambda src=src, dst=dst, b0=b0: nc.scalar.mul(out=dst[:, :, b0:b0 + 32],
                                                                                    in_=src[:, :, b0 + 32:b0 + 64], mul=-1.0),
                                      [b_w], [b_ws])
                                S.act(lambda src=src, dst=dst, b0=b0: nc.scalar.copy(out=dst[:, :, b0 + 32:b0 + 64],
                                                                                     in_=src[:, :, b0:b0 + 32]),
                                      [b_w], [b_ws])
                    for (w_, ws_, dst, bdst, scale) in ((wq, wqs, qT, b_q, 0.125), (wkk, wks, kT, b_k, 1.0)):
                        for tc in range(NQC):
                            cols = slice(tc * 512, (tc + 1) * 512)
                            pa = tc % 2
                            for k in range(8):
                                S.pe(lambda k=k, w_=w_, pa=pa, cols=cols: nc.tensor.matmul(
                                    PS[pa][:, :], lhsT=w_[:, k, :], rhs=hT[:, k, cols], start=(k == 0), stop=(k == 7)),
                                    [b_w, b_hT[tc]], [PB[pa]], sig=(k == 7))
                            if not moba:
                                for hh in range(2):
                                    S.act(lambda hh=hh, pa=pa, cols=cols, dst=dst, scale=scale: nc.scalar.mul(
                                        out=dst[hh][0:64, cols], in_=PS[pa][hh * 64:(hh + 1) * 64, :], mul=scale),
                                        [PB[pa]], [bdst[hh]])
                            else:
                                pb2 = 2 + tc % 2
                                for k in range(8):
                                    S.pe(lambda k=k, ws_=ws_, pb2=pb2, cols=cols: nc.tensor.matmul(
                                        PS[pb2][:, :], lhsT=ws_[:, k, :], rhs=hT[:, k, cols], start=(k == 0), stop=(k == 7)),
                                        [b_ws, b_hT[tc]], [PB[pb2]], sig=(k == 7))
                                S.dve(lambda pa=pa, cols=cols: nc.vector.tensor_tensor(out=t1[:], in0=PS[pa][:, :],
                                                                                       in1=cosT[:, cols], op=ALU.mult),
                                      [PB[pa], b_tab], [b_t1])
                                S.dve(lambda pb2=pb2, cols=cols: nc.vector.tensor_tensor(out=t2[:], in0=PS[pb2][:, :],
                                                                                         in1=sinT[:, cols], op=ALU.mult),
                                      [PB[pb2], b_tab], [b_t2])
                                S.pool(lambda: nc.gpsimd.tensor_tensor(out=t3[:], in0=t1[:], in1=t2[:], op=ALU.add),
                                       [b_t1, b_t2], [b_t3])
                                for hh in range(2):
                                    S.act(lambda hh=hh, cols=cols, dst=dst, scale=scale: nc.scalar.mul(
                                        out=dst[hh][0:64, cols], in_=t3[hh * 64:(hh + 1) * 64, :], mul=scale),
                                        [b_t3], [bdst[hh]])
                    for g4 in range(NT // 4):
                        pa = 4 + g4 % 2
                        for j in range(4):
                            tt = g4 * 4 + j
                            for k in range(8):
                                S.pe(lambda k=k, tt=tt, j=j, pa=pa: nc.tensor.matmul(
                                    PS[pa][:, j * 128:(j + 1) * 128], lhsT=hT[:, k, tt * 128:(tt + 1) * 128], rhs=wv[:, k, :],
                                    start=(k == 0), stop=(k == 7)), [b_w, b_hT[tt // 4]], [PB[pa]], sig=(j == 3 and k == 7))
                        S.act(lambda g4=g4, pa=pa: nc.scalar.activation(
                            out=vv[:, g4 * 4:(g4 + 1) * 4, :, 0:64],
                            in_=PS[pa][:, :].rearrange("p (j h d) -> p j h d", j=4, h=2), func=AF.Copy),
                            [PB[pa]], [b_v])

                def sb_head(hh, head):
                    q_, k_ = qT[hh], kT[hh]
                    pO = 4 + hh
                    e_, sp_, a_, ss_, o_ = e_sb[hh], sp_sb[hh], a_sb[hh], ssum[hh], o_sb[hh]
                    be_, bsp_, ba_, bss_, bo_ = b_e[hh], b_sp[hh], b_a[hh], b_ssum[hh], b_o[hh]
                    for qc in range(NQC):
                        kts = list(range(4 * qc + 3, -1, -1))
                        n_t = len(kts)

                        def par(idx):
                            kt = kts[idx]
                            j = kt - 4 * qc
                            return kt, j, (128 * j if j >= 0 else 0)

                        def st1(idx):
                            kt, j, c_lo = par(idx)
                            lc = slice(c_lo, 512)
                            tri = slice(c_lo, c_lo + 128)
                            qcols = slice(qc * 512 + c_lo, (qc + 1) * 512)
                            kcols = slice(kt * 128, (kt + 1) * 128)
                            i2 = idx % 2
                            pz = 2 * hh + i2
                            S.pe(lambda: nc.tensor.matmul(PS[pz][:, lc], lhsT=k_[0:64, kcols], rhs=q_[0:64, qcols],
                                                          start=True, stop=False), [b_k[hh], b_q[hh]], [PB[pz]], sig=True)
                            S.act(lambda: nc.scalar.activation(out=e_[i2][:, lc], in_=PS[pz][:, lc], func=AF.Exp),
                                  [PB[pz]], [be_[i2]])
                            S.act(lambda: nc.scalar.activation(out=sp_[i2][:, lc], in_=e_[i2][:, lc], func=AF.Ln,
                                                               bias=ones_f[:, 0:1], scale=1.0), [be_[i2], cb], [bsp_[i2]])
                            if j >= 0:
                                S.dve(lambda: nc.vector.tensor_tensor(out=sp_[i2][:, tri], in0=sp_[i2][:, tri], in1=m01[:],
                                                                      op=ALU.mult), [bsp_[i2], cb], [bsp_[i2]])
                            if idx + 1 < n_t:
                                nb_ = (idx + 1) % 2
                                if idx == 0:
                                    S.dve(lambda: nc.vector.tensor_copy(out=ss_[nb_][:, lc], in_=sp_[i2][:, lc]),
                                          [bsp_[i2]], [bss_[nb_]])
                                else:
                                    S.dve(lambda: nc.vector.tensor_tensor(out=ss_[nb_][:, lc], in0=ss_[idx % 2][:, lc],
                                                                          in1=sp_[i2][:, lc], op=ALU.add),
                                          [bsp_[i2], bss_[idx % 2]], [bss_[nb_]])
                                c_lo2 = par(idx + 1)[2]
                                if c_lo2 < c_lo:
                                    S.dve(lambda: nc.vector.memset(ss_[nb_][:, c_lo2:c_lo], 0.0), [], [bss_[nb_]])

                        def st2(idx):
                            kt, j, c_lo = par(idx)
                            lc = slice(c_lo, 512)
                            tri = slice(c_lo, c_lo + 128)
                            i2 = idx % 2
                            pB = 2 * hh + i2
                            first = idx == 0
                            S.pe(lambda: nc.tensor.matmul(PS[pB][:, lc], lhsT=tri_neg[:], rhs=sp_[i2][:, lc], start=False,
                                                          stop=False), [cb, bsp_[i2]], [PB[pB]])
                            if not first:
                                S.pe(lambda: nc.tensor.matmul(PS[pB][:, lc], lhsT=ones_neg[:], rhs=ss_[idx % 2][:, lc],
                                                              start=False, stop=(j < 0)), [cb, bss_[idx % 2]], [PB[pB]],
                                     sig=(j < 0))
                            if j >= 0:
                                S.pe(lambda: nc.tensor.matmul(PS[pB][:, tri], lhsT=ident_bf[:], rhs=nm_strict[:], start=False,
                                                              stop=True), [cb], [PB[pB]], sig=True)
                            S.act(lambda: nc.scalar.activation(out=a_[i2][:, lc], in_=PS[pB][:, lc], func=AF.Exp),
                                  [PB[pB]], [ba_[i2]])

                        def st3(idx):
                            kt, j, c_lo = par(idx)
                            lc = slice(c_lo, 512)
                            i2 = idx % 2
                            if idx == 0:
                                S.pe(lambda: nc.tensor.matmul(PS[pO][0:64, :], lhsT=zeros_bf[:, 0:64], rhs=zeros_bf[:, :],
                                                              start=True, stop=False), [cb], [PB[pO]])
                            S.pe(lambda: nc.tensor.matmul(PS[pO][0:64, lc], lhsT=vv[:, kt, hh, 0:64], rhs=a_[i2][:, lc],
                                                          start=False, stop=(idx == n_t - 1)), [b_v, ba_[i2]], [PB[pO]], sig=True)

                        for stp in range(n_t + 2):
                            if 0 <= stp - 2 < n_t:
                                st3(stp - 2)
                            if 0 <= stp - 1 < n_t:
                                st2(stp - 1)
                            if stp < n_t:
                                st1(stp)
                            yield
                        ob = qc % 2
                        S.act(lambda ob=ob: nc.scalar.activation(out=o_[ob][:, :], in_=PS[pO][0:64, :], func=AF.Copy),
                              [PB[pO]], [bo_[ob]])
                        S.dma("sp", lambda ob=ob, qc=qc: nc.sync.dma_start(
                            out=oT_d[head * 64:(head + 1) * 64, qc * 512:(qc + 1) * 512], in_=o_[ob][:, :]),
                            [bo_[ob]], [b_oT[head][qc]])
                        yield

                def moba_gate(hh, head):
                    q_, k_ = qT[hh], kT[hh]
                    S.dve(lambda: nc.vector.tensor_reduce(out=kmean[:], in_=k_[0:64, :].rearrange("p (n k) -> p n k", k=256),
                                                          axis=AX.X, op=ALU.add), [b_k[hh]], [b_km])
                    S.act(lambda: nc.scalar.copy(out=kmean_hi[:], in_=kmean[:]), [b_km], [b_km])
                    S.dve(lambda: nc.vector.tensor_tensor(out=kmr[:], in0=kmean[:], in1=kmean_hi[:], op=ALU.subtract),
                          [b_km], [b_km])
                    S.act(lambda: nc.scalar.copy(out=kmean_lo[:], in_=kmr[:]), [b_km], [b_km])
                    for tt in range(NT):
                        own = tt // 2
                        i2 = tt % 2
                        pg = i2
                        tcols = slice(tt * 128, (tt + 1) * 128)
                        S.dve(lambda i2=i2: nc.vector.memset(xaug[i2][:], 0.0), [], [b_g[i2]])
                        if own >= 4:
                            S.pe(lambda pg=pg, tcols=tcols: nc.tensor.matmul(PS[pg][:, 0:16], lhsT=q_[0:64, tcols],
                                                                            rhs=kmean_hi[:], start=True, stop=False),
                                 [b_q[hh], b_km], [PB[pg]])
                            S.pe(lambda pg=pg, tcols=tcols: nc.tensor.matmul(PS[pg][:, 0:16], lhsT=q_[0:64, tcols],
                                                                            rhs=kmean_lo[:], start=False, stop=True),
                                 [b_q[hh], b_km], [PB[pg]], sig=True)
                            S.dve(lambda i2=i2: nc.vector.memset(gpad[i2][:], -1e30), [], [b_g[i2]])
                            S.dve(lambda i2=i2, pg=pg, own=own: nc.vector.tensor_copy(out=gpad[i2][:, 0:own],
                                                                                      in_=PS[pg][:, 0:own]),
                                  [PB[pg]], [b_g[i2]])
                            S.dve(lambda i2=i2: nc.vector.max(out=top8[i2][:], in_=gpad[i2][:]), [b_g[i2]], [b_g[i2]])
                            S.dve(lambda i2=i2, own=own: nc.vector.tensor_scalar(
                                out=xaug[i2][:, 1:1 + own], in0=gpad[i2][:, 0:own], scalar1=top8[i2][:, 2:3], scalar2=None,
                                op0=ALU.is_ge), [b_g[i2]], [b_g[i2]])
                            S.dve(lambda i2=i2, own=own: nc.vector.tensor_scalar(
                                out=xaug[i2][:, 1:1 + own], in0=xaug[i2][:, 1:1 + own], scalar1=-1.0, scalar2=BIG,
                                op0=ALU.add, op1=ALU.mult), [b_g[i2]], [b_g[i2]])
                        pt = 2 + i2
                        S.pe(lambda pt=pt, i2=i2: nc.tensor.matmul(PS[pt][0:32, 0:128], lhsT=xaug[i2][:, :], rhs=ident_f[:],
                                                                   start=True, stop=True), [b_g[i2], cb], [PB[pt]], sig=True)
                        S.act(lambda pt=pt, tcols=tcols: nc.scalar.activation(out=q_[64:96, tcols], in_=PS[pt][0:32, 0:128],
                                                                              func=AF.Copy), [PB[pt]], [b_q[hh]])

                def moba_head(hh, head):
                    q_, k_ = qT[hh], kT[hh]
                    a_, o_, ba_, bo_ = a_sb[hh], o_sb[hh], b_a[hh], b_o[hh]
                    for qc in range(NQC):
                        pO = 4 + hh
                        nk = 4 * qc + 4

                        def m1(kt):
                            j = kt - 4 * qc
                            c_lo = 128 * j if j >= 0 else 0
                            qcols = slice(qc * 512 + c_lo, (qc + 1) * 512)
                            lc = slice(c_lo, 512)
                            tri = slice(c_lo, c_lo + 128)
                            kcols = slice(kt * 128, (kt + 1) * 128)
                            i2 = kt % 2
                            pz = 2 * hh + i2
                            S.pe(lambda: nc.tensor.matmul(PS[pz][:, lc], lhsT=k_[0:96, kcols], rhs=q_[0:96, qcols], start=True,
                                                          stop=(j < 0)), [b_k[hh], b_q[hh]], [PB[pz]], sig=(j < 0))
                            if j >= 0:
                                S.pe(lambda: nc.tensor.matmul(PS[pz][:, tri], lhsT=ident_bf[:], rhs=nm_causal[:], start=False,
                                                              stop=True), [cb], [PB[pz]], sig=True)
                            S.act(lambda: nc.scalar.activation(out=a_[i2][:, lc], in_=PS[pz][:, lc], func=AF.Exp),
                                  [PB[pz]], [ba_[i2]])

                        def m2(kt):
                            j = kt - 4 * qc
                            c_lo = 128 * j if j >= 0 else 0
                            lc = slice(c_lo, 512)
                            i2 = kt % 2
                            S.pe(lambda: nc.tensor.matmul(PS[pO][0:65, lc], lhsT=vv[:, kt, hh, :], rhs=a_[i2][:, lc],
                                                          start=(kt == 0), stop=(kt == nk - 1)), [b_v, ba_[i2]], [PB[pO]], sig=True)

                        for stp in range(nk + 1):
                            if 0 <= stp - 1 < nk:
                                m2(stp - 1)
                            if stp < nk:
                                m1(stp)
                            yield
                        S.dve(lambda: nc.vector.reciprocal(out=rden[hh][0:1, :], in_=PS[pO][64:65, :]), [PB[pO]], [b_rden[hh]])
                        S.act(lambda: nc.scalar.copy(out=of_sb[hh][:, :], in_=PS[pO][0:64, :]), [PB[pO]], [b_of[hh]])
                        pb = 6 + hh
                        S.pe(lambda: nc.tensor.matmul(PS[pb][0:64, :], lhsT=ones_row[0:1, :], rhs=rden[hh][0:1, :],
                                                      start=True, stop=True), [cb, b_rden[hh]], [PB[pb]], sig=True)
                        ob = qc % 2
                        S.dve(lambda ob=ob: nc.vector.tensor_tensor(out=o_[ob][:, :], in0=of_sb[hh][:, :], in1=PS[pb][0:64, :],
                                                                    op=ALU.mult), [b_of[hh], PB[pb]], [bo_[ob]])
                        S.dma("sp", lambda ob=ob, qc=qc: nc.sync.dma_start(
                            out=oT_d[head * 64:(head + 1) * 64, qc * 512:(qc + 1) * 512], in_=o_[ob][:, :]),
                            [bo_[ob]], [b_oT[head][qc]])
                        yield

                pairs = list(range(8))
                if stop_after == "sb0":
                    pairs = [0]
                if stop_after == "moba0":
                    pairs = [4]
                for hp in pairs:
                    moba = hp >= 4
                    project_pair(hp, moba)
                    if moba:
                        for hh in range(2):
                            moba_gate(hh, hp * 2 + hh)
                        gens = [moba_head(hh, hp * 2 + hh) for hh in range(2)]
                    else:
                        gens = [sb_head(hh, hp * 2 + hh) for hh in range(2)]
                    while gens:
                        for g_ in list(gens):
                            try:
                                next(g_)
                            except StopIteration:
                                gens.remove(g_)
                if debug:
                    allo = [b for row in b_oT for b in row]
                    S.dma("sp", lambda: nc.sync.dma_start(out=dbg["oT"][:, :], in_=oT_d[:, :]), allo, [], is_out=True)
                S.barrier()

        b_xs = [Buf("xs%d" % i) for i in range(NT)]

        def phase_outproj(x_src, mixT_d, FC, w_src):
            with contextlib.ExitStack() as ph:
                psb = lambda name, shape, dt: ph.enter_context(sbt(name, shape, dt))
                wo = psb("wo", [128, FC, D], BF16)
                b_wo = Buf()
                S.dma("pool", lambda: nc.gpsimd.dma_start(out=wo[:], in_=w_src.rearrange("(c p) n -> p c n", p=128)), [], [b_wo])
                mix = [psb("mix%d" % i, [128, FC, 512], BF16) for i in range(2)]
                b_mix = [Buf(), Buf()]
                xt = [psb("o_xt%d" % i, [128, D], F32) for i in range(2)]
                tmp = [psb("o_tmp%d" % i, [128, D], F32) for i in range(2)]
                b_xt, b_tmp = [Buf(), Buf()], [Buf(), Buf()]
                mv = mixT_d.rearrange("(c p) t -> p c t", p=128)

                def load_mix(c):
                    S.dma("sp", lambda: nc.sync.dma_start(out=mix[c % 2][:], in_=mv[:, :, c * 512:(c + 1) * 512]),
                          [], [b_mix[c % 2]])

                def load_x(tt):
                    S.dma("sp", lambda: nc.sync.dma_start(out=xt[tt % 2][:], in_=x_src[tt * 128:(tt + 1) * 128, :]),
                          [b_xs[tt]], [b_xt[tt % 2]])

                load_mix(0)
                load_x(0)
                for c in range(NQC):
                    for j in range(4):
                        tt = c * 4 + j
                        i2 = tt % 2
                        for half in range(2):
                            pb = half + 2 * i2
                            hs = slice(half * 512, (half + 1) * 512)
                            for cc in range(FC):
                                S.pe(lambda cc=cc, c=c, j=j, pb=pb, hs=hs: nc.tensor.matmul(
                                    PS[pb][:, :], lhsT=mix[c % 2][:, cc, j * 128:(j + 1) * 128], rhs=wo[:, cc, hs],
                                    start=(cc == 0), stop=(cc == FC - 1)), [b_mix[c % 2], b_wo], [PB[pb]], sig=(cc == FC - 1))
                            S.dve(lambda i2=i2, pb=pb, hs=hs: nc.vector.tensor_tensor(out=tmp[i2][:, hs], in0=PS[pb][:, :],
                                                                                      in1=MOD[G_M][:, hs], op=ALU.mult),
                                  [PB[pb], modb[G_M]], [b_tmp[i2]])
                        S.pool(lambda i2=i2: nc.gpsimd.tensor_tensor(out=xt[i2][:], in0=tmp[i2][:], in1=xt[i2][:], op=ALU.add),
                               [b_tmp[i2], b_xt[i2]], [b_xt[i2]])
                        if tt + 1 < NT:
                            load_x(tt + 1)
                        if j == 0 and c + 1 < NQC:
                            load_mix(c + 1)
                        S.dma("sp", lambda tt=tt, i2=i2: nc.sync.dma_start(out=xs[tt * 128:(tt + 1) * 128, :], in_=xt[i2][:]),
                              [b_xt[i2]], [b_xs[tt]])
                S.barrier()

        def phase_ffn(layer, final):
            with contextlib.ExitStack() as ph:
                psb = lambda name, shape, dt: ph.enter_context(sbt(name, shape, dt))
                wgu = psb("wgu", [128, 8, 2 * DFF], BF16)
                wd = psb("wd", [128, NFC, D], BF16)
                b_wgu, b_wd = Buf(), Buf()
                gsrc = I["ffn_w_gate_up"][layer].rearrange("(k p) n -> p k n", p=128)
                for k0 in (0, 4):
                    S.dma("pool", lambda k0=k0: nc.gpsimd.dma_start(out=wgu[:, k0:k0 + 4, :], in_=gsrc[:, k0:k0 + 4, :]),
                          [], [b_wgu])
                dsrc = I["ffn_w_down"][layer].rearrange("(c p) n -> p c n", p=128)
                for f0 in range(0, NFC, 11):
                    S.dma("pool", lambda f0=f0: nc.gpsimd.dma_start(out=wd[:, f0:f0 + 11, :], in_=dsrc[:, f0:f0 + 11, :]),
                          [], [b_wd])
                wk = make_norm_wk(ph, "f_", psT=(4, 5), single=True)
                h2T = psb("h2T", [128, 8, 512], BF16)
                b_h2T = Buf()
                actT = psb("actT", [128, NFC, 512], BF16)
                b_act = [Buf() for _ in range(NFC)]
                sg0 = psb("sg0", [128, 512], F32)
                sg = [sg0, sg0]
                bsg0 = Buf()
                b_sg = [bsg0, bsg0]
                xt = [psb("f_xt%d" % i, [128, D], F32) for i in range(2)]
                b_xt = [Buf(), Buf()]
                tmp = wk["tmp"][0]
                b_tmp = wk["b_tmp"][0]
                if final:
                    fn = MOD[A_M]
                    b_fn = modb[A_M]
                    S.dma("sp", lambda: nc.sync.dma_start(out=fn[:], in_=I["final_norm"][0:1, :].partition_broadcast(128)),
                          [], [b_fn])
                    fss = psb("fss", [128, 1], F32)
                    frs = psb("frs", [128, 1], F32)
                    b_fs = Buf()
                for c in range(NQC):
                    for j in range(4):
                        tt = c * 4 + j
                        i2 = tt % 2
                        S.dma("sp", lambda tt=tt, i2=i2: nc.sync.dma_start(out=xt[i2][:], in_=xs[tt * 128:(tt + 1) * 128, :]),
                              [b_xs[tt]], [b_xt[i2]])
                        norm_mod_transpose(lambda t: xt[t % 2][:], lambda t: b_xt[t % 2], A_F, B_F, h2T,
                                           lambda t: b_h2T, lambda t: (t % 4) * 128, [tt], wk)
                    for f in range(NFC):
                        pg, pu = (0, 1) if f % 2 == 0 else (2, 3)
                        for k in range(8):
                            S.pe(lambda k=k, f=f, pg=pg: nc.tensor.matmul(PS[pg][:, :], lhsT=wgu[:, k, f * 128:(f + 1) * 128],
                                                                         rhs=h2T[:, k, :], start=(k == 0), stop=(k == 7)),
                                 [b_wgu, b_h2T], [PB[pg]], sig=(k == 7))
                        for k in range(8):
                            S.pe(lambda k=k, f=f, pu=pu: nc.tensor.matmul(
                                PS[pu][:, :], lhsT=wgu[:, k, DFF + f * 128:DFF + (f + 1) * 128], rhs=h2T[:, k, :],
                                start=(k == 0), stop=(k == 7)), [b_wgu, b_h2T], [PB[pu]], sig=(k == 7))
                        S.act(lambda f=f, pg=pg: nc.scalar.activation(out=sg[f % 2][:], in_=PS[pg][:, :], func=AF.Silu),
                              [PB[pg]], [b_sg[f % 2]])
                        S.dve(lambda f=f, pu=pu: nc.vector.tensor_tensor(out=actT[:, f, :], in0=sg[f % 2][:], in1=PS[pu][:, :],
                                                                         op=ALU.mult), [b_sg[f % 2], PB[pu]], [b_act[f]])
                    for j in range(4):
                        tt = c * 4 + j
                        i2 = tt % 2
                        S.dma("sp", lambda tt=tt, i2=i2: nc.sync.dma_start(out=xt[i2][:], in_=xs[tt * 128:(tt + 1) * 128, :]),
                              [b_xs[tt]], [b_xt[i2]])
                        for half in range(2):
                            pb = 6 + half
                            hs = slice(half * 512, (half + 1) * 512)
                            for f in range(NFC):
                                S.pe(lambda f=f, j=j, pb=pb, hs=hs: nc.tensor.matmul(
                                    PS[pb][:, :], lhsT=actT[:, f, j * 128:(j + 1) * 128], rhs=wd[:, f, hs],
                                    start=(f == 0), stop=(f == NFC - 1)), [b_act[f], b_wd], [PB[pb]], sig=(f == NFC - 1))
                            S.dve(lambda pb=pb, hs=hs: nc.vector.tensor_tensor(out=tmp[:, hs], in0=PS[pb][:, :],
                                                                               in1=MOD[G_F][:, hs], op=ALU.mult),
                                  [PB[pb], modb[G_F]], [b_tmp])
                        S.pool(lambda i2=i2: nc.gpsimd.tensor_tensor(out=xt[i2][:], in0=tmp[:], in1=xt[i2][:], op=ALU.add),
                               [b_tmp, b_xt[i2]], [b_xt[i2]])
                        if not final:
                            S.dma("sp", lambda tt=tt, i2=i2: nc.sync.dma_start(out=xs[tt * 128:(tt + 1) * 128, :], in_=xt[i2][:]),
                                  [b_xt[i2]], [b_xs[tt]])
                        else:
                            S.dve(lambda: nc.vector.memset(fss[:], 0.0), [], [b_fs])
                            S.act(lambda i2=i2: nc.scalar.activation(out=wk["junk"][:], in_=xt[i2][:], func=AF.Square,
                                                                     accum_out=fss[:, 0:1]), [b_xt[i2]], [wk["b_junk"], b_fs])
                            S.act(lambda: nc.scalar.activation(out=frs[:], in_=fss[:], func=AF.Sqrt, bias=eps6[:, 0:1],
                                                               scale=1.0 / D), [b_fs, cb], [b_fs])
                            S.dve(lambda: nc.vector.reciprocal(out=frs[:], in_=frs[:]), [b_fs], [b_fs])
                            S.dve(lambda i2=i2: nc.vector.scalar_tensor_tensor(out=tmp[:], in0=xt[i2][:], scalar=frs[:, 0:1],
                                                                               in1=fn[:], op0=ALU.mult, op1=ALU.mult),
                                  [b_xt[i2], b_fs, b_fn], [b_tmp])
                            S.dma("sp", lambda tt=tt: nc.sync.dma_start(out=y_out[tt * 128:(tt + 1) * 128, :], in_=tmp[:]),
                                  [b_tmp], [b_xs[tt]], is_out=True)
                S.barrier()

        def phase_ret():
            gam = [1.0 - 2.0 ** (-5.0 - h) for h in range(4)]
            with contextlib.ExitStack() as ph:
                psb = lambda name, shape, dt: ph.enter_context(sbt(name, shape, dt))
                hT = psb("r_hT", [128, 8, S_LEN], BF16)
                b_hT = [Buf() for _ in range(NQC)]
                with contextlib.ExitStack() as ph1:
                    wk = make_norm_wk(ph1, "r_")
                    xt = [ph1.enter_context(sbt("r_xt%d" % i, [128, D], F32)) for i in range(2)]
                    b_xt = [Buf(), Buf()]
                    ag = ada_gen(1, ph1)
                    for _ in range(4):
                        next(ag)
                    for tt in range(NT):
                        S.dma("sp", lambda tt=tt: nc.sync.dma_start(out=xt[tt % 2][:], in_=xs[tt * 128:(tt + 1) * 128, :]),
                              [b_xs[tt]], [b_xt[tt % 2]])
                        norm_mod_transpose(lambda t: xt[t % 2][:], lambda t: b_xt[t % 2], A_M, B_M, hT,
                                           lambda t: b_hT[t // 4], lambda t: t * 128, [tt], wk)
                        if tt % 4 == 3:
                            next(ag, None)
                    for _ in ag:
                        pass
                    S.barrier()
                cosT = psb("r_cos", [128, S_LEN], BF16)
                sinT = psb("r_sin", [128, S_LEN], BF16)
                b_tab = Buf()
                rope_tables(ph, "r_", "inv_ret", cosT, sinT, b_tab)
                wqk = psb("r_wqk", [128, 8, 512], BF16)
                wv = psb("r_wv", [128, 8, 512], BF16)
                b_wqk, b_wv = Buf(), Buf()
                qT = psb("r_qT", [128, 2, S_LEN], BF16)
                kT = psb("r_kT", [128, 2, S_LEN], BF16)
                vt = psb("r_v", [128, NT, 512], BF16)
                b_q, b_k, b_v = Buf(), Buf(), Buf()
                Gf = psb("r_Gf", [128, 512], F32)
                Gm = psb("r_Gm", [128, 512], F32)
                gn = psb("r_gn", [128, 512], F32)
                b_G = Buf()
                t1 = psb("r_t1", [128, 512], F32)
                t2 = psb("r_t2", [128, 512], F32)
                b_t1, b_t2 = Buf(), Buf()
                p_sb = [psb("r_p%d" % i, [128, 512], BF16) for i in range(2)]
                b_p = [Buf(), Buf()]
                o_sb, g_sb = t1, t2
                u_sb = psb("r_u", [128, 512], BF16)
                uTq = [psb("r_uT%d" % i, [128, 4, 128], BF16) for i in range(2)]
                b_uTq = [Buf(), Buf()]
                b_o, b_g, b_u = b_t1, b_t2, Buf()
                oq = [psb("r_oq%d" % i, [128, 512], BF16) for i in range(4)]
                b_oq = [Buf() for _ in range(4)]
                stq = [psb("r_st%d" % i, [128, 8], F32) for i in range(4)]
                b_stq = [Buf() for _ in range(4)]
                wsrc = I["ret_w_in"].rearrange("(k p) n -> p k n", p=128)
                for h in range(4):
                    S.dma("pool", lambda h=h: nc.gpsimd.dma_start(out=wqk[:, :, 0:256], in_=wsrc[:, :, h * 256:(h + 1) * 256]),
                          [], [b_wqk])
                    S.dma("pool", lambda h=h: nc.gpsimd.dma_start(out=wqk[:, :, 256:512],
                                                                  in_=wsrc[:, :, D + h * 256:D + (h + 1) * 256]), [], [b_wqk])
                    S.dma("pool", lambda h=h: nc.gpsimd.dma_start(out=wv[:], in_=wsrc[:, :, 2 * D + h * 512:2 * D + (h + 1) * 512]),
                          [], [b_wv])
                    S.dma("sp", lambda h=h: nc.sync.dma_start(out=Gf[:], in_=C["ret_gf"][h]), [], [b_G])
                    S.dma("sp", lambda h=h: nc.sync.dma_start(out=Gm[:], in_=C["ret_gm"][h]), [], [b_G])
                    S.dma("sp", lambda h=h: nc.sync.dma_start(
                        out=gn[:], in_=I["ret_gn"][0:1, h * 512:(h + 1) * 512].partition_broadcast(128)), [], [b_G])
                    for (c0, dst, bdst) in ((0, qT, b_q), (256, kT, b_k)):
                        for tc in range(NQC):
                            cols = slice(tc * 512, (tc + 1) * 512)
                            for dc in range(2):
                                for k in range(8):
                                    S.pe(lambda k=k, dc=dc, c0=c0, cols=cols: nc.tensor.matmul(
                                        PS[dc][:, :], lhsT=wqk[:, k, c0 + dc * 128:c0 + (dc + 1) * 128], rhs=hT[:, k, cols],
                                        start=(k == 0), stop=(k == 7)), [b_wqk, b_hT[tc]], [PB[dc]], sig=(k == 7))
                            S.dve(lambda cols=cols: nc.vector.tensor_tensor(out=t1[:], in0=PS[0][:, :], in1=cosT[:, cols],
                                                                            op=ALU.mult), [PB[0], b_tab], [b_t1])
                            S.dve(lambda cols=cols: nc.vector.tensor_tensor(out=t2[:], in0=PS[1][:, :], in1=sinT[:, cols],
                                                                            op=ALU.mult), [PB[1], b_tab], [b_t2])
                            S.pool(lambda cols=cols, dst=dst: nc.gpsimd.tensor_tensor(out=dst[:, 0, cols], in0=t1[:], in1=t2[:],
                                                                                      op=ALU.subtract), [b_t1, b_t2], [bdst])
                            S.dve(lambda cols=cols: nc.vector.tensor_tensor(out=t1[:], in0=PS[1][:, :], in1=cosT[:, cols],
                                                                            op=ALU.mult), [PB[1], b_tab], [b_t1])
                            S.dve(lambda cols=cols: nc.vector.tensor_tensor(out=t2[:], in0=PS[0][:, :], in1=sinT[:, cols],
                                                                            op=ALU.mult), [PB[0], b_tab], [b_t2])
                            S.pool(lambda cols=cols, dst=dst: nc.gpsimd.tensor_tensor(out=dst[:, 1, cols], in0=t1[:], in1=t2[:],
                                                                                      op=ALU.add), [b_t1, b_t2], [bdst])
                    for tt in range(NT):
                        pa = 2 + tt % 2
                        for k in range(8):
                            S.pe(lambda k=k, tt=tt, pa=pa: nc.tensor.matmul(
                                PS[pa][:, :], lhsT=hT[:, k, tt * 128:(tt + 1) * 128], rhs=wv[:, k, :], start=(k == 0),
                                stop=(k == 7)), [b_wv, b_hT[tt // 4]], [PB[pa]], sig=(k == 7))
                        S.act(lambda tt=tt, pa=pa: nc.scalar.copy(out=vt[:, tt, :], in_=PS[pa][:, :]), [PB[pa]], [b_v])
                    S.dma("pool", lambda h=h: nc.gpsimd.dma_start(out=wqk[:], in_=wsrc[:, :, 4 * D + h * 512:4 * D + (h + 1) * 512]),
                          [], [b_wqk])
                    def ret_main(qc):
                        nk = 4 * qc + 4

                        def r1(kt):
                            j = kt - 4 * qc
                            c_lo = 128 * j if j >= 0 else 0
                            n = 512 - c_lo
                            lc = slice(c_lo, 512)
                            qcols = slice(qc * 512 + c_lo, (qc + 1) * 512)
                            kcols = slice(kt * 128, (kt + 1) * 128)
                            pz = kt % 2
                            for dc in range(2):
                                S.pe(lambda dc=dc: nc.tensor.matmul(PS[pz][:, lc], lhsT=kT[:, dc, kcols], rhs=qT[:, dc, qcols],
                                                                    start=(dc == 0), stop=(dc == 1)), [b_k, b_q], [PB[pz]],
                                     sig=(dc == 1))
                            scal = 1.0 if j >= 0 else float(gam[h] ** (128.0 * (4 * qc - kt)))
                            G_ = Gm if j >= 0 else Gf
                            S.dve(lambda: nc.vector.scalar_tensor_tensor(out=p_sb[pz][:, lc], in0=PS[pz][:, lc], scalar=scal,
                                                                         in1=G_[:, 0:n], op0=ALU.mult, op1=ALU.mult),
                                  [PB[pz], b_G], [b_p[pz]])

                        def r2(kt):
                            j = kt - 4 * qc
                            pz = kt % 2
                            for qt in range(max(j, 0), 4):
                                S.pe(lambda qt=qt: nc.tensor.matmul(PS[2 + qt][:, :], lhsT=p_sb[pz][:, qt * 128:(qt + 1) * 128],
                                                                    rhs=vt[:, kt, :], start=(kt == 0), stop=(kt == 4 * qc + qt)),
                                     [b_p[pz], b_v], [PB[2 + qt]], sig=True)

                        for stp in range(nk + 1):
                            if 0 <= stp - 1 < nk:
                                r2(stp - 1)
                            if stp < nk:
                                r1(stp)
                            yield

                    def ret_epi(qc):
                        for qt in range(4):
                            po = 2 + qt
                            S.dve(lambda qt=qt: nc.vector.memset(stq[qt][:], 0.0), [], [b_stq[qt]])
                            S.act(lambda qt=qt, po=po: nc.scalar.activation(out=oq[qt][:], in_=PS[po][:, :], func=AF.Copy,
                                                                            accum_out=stq[qt][:, 0:1]), [PB[po]],
                                  [b_oq[qt], b_stq[qt]])
                        yield
                        for qt in range(4):
                            tt = 4 * qc + qt
                            st = stq[qt]
                            b_st = b_stq[qt]
                            S.act(lambda qt=qt, st=st: nc.scalar.activation(out=g_sb[:], in_=oq[qt][:], func=AF.Square,
                                                                            accum_out=st[:, 1:2]), [b_oq[qt]], [b_g, b_st])
                            S.dve(lambda st=st: nc.vector.tensor_scalar(out=st[:, 2:3], in0=st[:, 0:1], scalar1=1.0 / 512,
                                                                        scalar2=None, op0=ALU.mult), [b_st], [b_st])
                            S.dve(lambda st=st: nc.vector.tensor_tensor(out=st[:, 3:4], in0=st[:, 2:3], in1=st[:, 2:3],
                                                                        op=ALU.mult), [b_st], [b_st])
                            S.dve(lambda st=st: nc.vector.scalar_tensor_tensor(out=st[:, 4:5], in0=st[:, 1:2], scalar=1.0 / 512,
                                                                               in1=st[:, 3:4], op0=ALU.mult, op1=ALU.subtract),
                                  [b_st], [b_st])
                            yield
                            S.act(lambda st=st: nc.scalar.activation(out=st[:, 5:6], in_=st[:, 4:5], func=AF.Sqrt,
                                                                     bias=eps5[:, 0:1], scale=1.0), [b_st, cb], [b_st])
                            S.dve(lambda st=st: nc.vector.reciprocal(out=st[:, 6:7], in_=st[:, 5:6]), [b_st], [b_st])
                            S.dve(lambda qt=qt, st=st: nc.vector.tensor_scalar(out=o_sb[:], in0=oq[qt][:], scalar1=st[:, 2:3],
                                                                               scalar2=st[:, 6:7], op0=ALU.subtract, op1=ALU.mult),
                                  [b_oq[qt], b_st], [b_o])
                            S.pool(lambda: nc.gpsimd.tensor_tensor(out=o_sb[:], in0=o_sb[:], in1=gn[:], op=ALU.mult),
                                   [b_o, b_G], [b_o])
                            yield
                            for k in range(8):
                                S.pe(lambda k=k, tt=tt: nc.tensor.matmul(PS[6][:, :], lhsT=hT[:, k, tt * 128:(tt + 1) * 128],
                                                                        rhs=wqk[:, k, :], start=(k == 0), stop=(k == 7)),
                                     [b_wqk, b_hT[tt // 4]], [PB[6]], sig=(k == 7))
                            S.act(lambda: nc.scalar.activation(out=g_sb[:], in_=PS[6][:, :], func=AF.Silu), [PB[6]], [b_g])
                            S.dve(lambda: nc.vector.tensor_tensor(out=u_sb[:], in0=o_sb[:], in1=g_sb[:], op=ALU.mult),
                                  [b_o, b_g], [b_u])
                            yield
                            pst = PS[7].bitcast(BF16)
                            for jj in range(4):
                                S.pe(lambda jj=jj, pst=pst: nc.tensor.transpose(out=pst[:, jj * 128:(jj + 1) * 128],
                                                                               in_=u_sb[:, jj * 128:(jj + 1) * 128],
                                                                               identity=ident_bf[:]), [b_u, cb], [PB[7]], sig=(jj == 3))
                            ub = qt % 2
                            S.act(lambda ub=ub, pst=pst: nc.scalar.activation(
                                out=uTq[ub][:, :, :], in_=pst[:, 0:512].rearrange("p (j t) -> p j t", j=4), func=AF.Copy),
                                [PB[7]], [b_uTq[ub]])
                            S.dma("sp", lambda ub=ub, tt=tt: nc.sync.dma_start(
                                out=uT_d[h * 512:(h + 1) * 512, tt * 128:(tt + 1) * 128].rearrange("(j p) t -> p j t", p=128),
                                in_=uTq[ub][:]), [b_uTq[ub]], [])
                            yield

                    gens = [ret_main(0)]
                    for qc in range(NQC):
                        while gens:
                            for g_ in list(gens):
                                try:
                                    next(g_)
                                except StopIteration:
                                    gens.remove(g_)
                        gens = [ret_epi(qc)]
                        if qc + 1 < NQC:
                            gens.append(ret_main(qc + 1))
                    while gens:
                        for g_ in list(gens):
                            try:
                                next(g_)
                            except StopIteration:
                                gens.remove(g_)
                S.barrier()

        phase_att()
        if stop_after in (None, "l0"):
            phase_outproj(I["x"], oT_d, 8, I["att_w_o"])
            phase_ffn(0, final=(stop_after == "l0"))
        if stop_after is None:
            phase_ret()
            phase_outproj(xs, uT_d, 16, I["ret_w_o"])
            phase_ffn(1, final=True)
        S.finish()
    return nc, S


def _prep_inputs(inputs, b):
    m = {}
    f = lambda a: np.ascontiguousarray(np.asarray(a, dtype=np.float32))
    m["x"] = f(inputs["x"][b])
    m["c"] = f(inputs["c"][b]).reshape(128, 8)
    m["ada_w"] = f(inputs["ada_w"])
    m["ada_b"] = f(inputs["ada_b"])
    m["norm_gains"] = f(inputs["norm_gains"])
    m["att_w_qkv"] = f(inputs["att_w_qkv"][0])
    m["att_w_o"] = f(inputs["att_w_o"][0])
    m["ret_w_in"] = f(inputs["ret_w_in"][0])
    m["ret_gn"] = f(inputs["ret_gn"]).reshape(1, 2 * D)
    m["ret_w_o"] = f(inputs["ret_w_o"][0])
    m["ffn_w_gate_up"] = f(inputs["ffn_w_gate_up"])
    m["ffn_w_down"] = f(inputs["ffn_w_down"])
    m["final_norm"] = f(inputs["final_norm"]).reshape(1, D)
    for k, v in _consts().items():
        if k == "ret_cd":
            continue
        m["k_" + k] = np.ascontiguousarray(v)
    return m


def kernel(**inputs):
    nc, _ = build_program()
    maps = [_prep_inputs(inputs, b) for b in range(4)]
    res = run_bass_kernel_spmd(nc, maps, core_ids=list(range(4)))
    out = np.stack([np.asarray(res.results[b]["y"], dtype=np.float32) for b in range(4)], axis=0)
    return out
```
